# Optimizing a Trainium2 kernel written in Bass

```python
import jax
import jax.numpy as jnp
from jax import lax
import numpy as np

D_MODEL = 1024
BATCH = 32
SEQ = 2048
DEPTH = 1

GRID_W = 64
CTX_LEN = 256
N_MOD = 6
F_GROUPS = 4
F_GROUP_DIM = 128
F_WIDTH = F_GROUPS * F_GROUP_DIM
HG_HEADS = 4
HG_DK = 128
HG_DV = 128
HG_KEY_WIDTH = HG_HEADS * HG_DK
HG_WIDTH = HG_HEADS * HG_DV
CHUNK = 64
N_BRANCHES = 2
SPLIT_POINTS = (F_WIDTH, F_WIDTH + HG_KEY_WIDTH, F_WIDTH + 2 * HG_KEY_WIDTH, F_WIDTH + 3 * HG_KEY_WIDTH, F_WIDTH + 3 * HG_KEY_WIDTH + HG_WIDTH, F_WIDTH + 3 * HG_KEY_WIDTH + 2 * HG_WIDTH)
IN_WIDTH = SPLIT_POINTS[-1] + N_BRANCHES * D_MODEL
N_EXPERTS = 32
TOP_K = 4
D_EXPERT = D_MODEL
SWIGLU_LIMIT = 7.0
SWIGLU_ALPHA = 1.702
MOE_BLOCK = 256
DEEPNORM_ALPHA = (2.0 * DEPTH) ** 0.25
DEEPNORM_BETA = (8.0 * DEPTH) ** -0.25
LN_EPS = 1e-5
RMS_EPS = 1e-6

kernel_name = 'hybrid_fourier_hgrn2_moe_dit_block'


def _layer_norm(x, g, b):
    xf = x.astype(jnp.float32)
    mu = jnp.mean(xf, axis=-1, keepdims=True)
    xc = xf - mu
    var = jnp.mean(xc * xc, axis=-1, keepdims=True)
    return (xc * lax.rsqrt(var + LN_EPS) * g.astype(jnp.float32) + b.astype(jnp.float32)).astype(x.dtype)


def _split_in(p):
    return jnp.split(p, SPLIT_POINTS, axis=-1)


def _fourier_latent(t):
    b, length, _ = t.shape
    rows = length // GRID_W
    g = t.astype(jnp.float32).reshape(b, rows, GRID_W, F_GROUPS, F_GROUP_DIM)
    y = jnp.fft.fftn(g, axes=(1, 2, 4), norm='ortho').real
    return y.reshape(b, length, F_WIDTH).astype(t.dtype)


def _fourier_context(t):
    b, length, _ = t.shape
    g = t.astype(jnp.float32).reshape(b, length, F_GROUPS, F_GROUP_DIM)
    y = jnp.fft.fftn(g, axes=(1, 3), norm='ortho').real
    return y.reshape(b, length, F_WIDTH).astype(t.dtype)


def _to_heads(t, d):
    b, length, _ = t.shape
    return t.reshape(b, length, HG_HEADS, d).transpose(0, 2, 1, 3)


def _forget_gate(z, lb):
    f = lb + (1.0 - lb) * jax.nn.sigmoid(z.astype(jnp.float32))
    return _to_heads(1.0 - f, HG_DK), _to_heads(jnp.log(f), HG_DK)


def _gla_chunked(q, k, v, log_f, s0):
    b, h, length, dk = q.shape
    dv = v.shape[-1]
    n = length // CHUNK
    q, k, log_f = (t.reshape(b, h, n, CHUNK, dk) for t in (q, k, log_f))
    v = v.reshape(b, h, n, CHUNK, dv)
    cum = jnp.cumsum(log_f, axis=3)
    cum_last = cum[:, :, :, -1:, :]
    q_dec = q * jnp.exp(cum)
    k_inv = k * jnp.exp(-cum)
    k_end = k * jnp.exp(cum_last - cum)
    scores = jnp.einsum('bhncd,bhnsd->bhncs', q_dec, k_inv)
    tri = jnp.tril(jnp.ones((CHUNK, CHUNK), dtype=bool))
    scores = jnp.where(tri, scores, 0.0)
    o_intra = jnp.einsum('bhncs,bhnse->bhnce', scores, v)
    chunk_decay = jnp.exp(cum_last[:, :, :, 0, :])

    def step(state, inp):
        q_c, k_c, v_c, g_c = inp
        o_c = jnp.einsum('bhcd,bhde->bhce', q_c, state)
        state = g_c[..., None] * state + jnp.einsum('bhcd,bhce->bhde', k_c, v_c)
        return state, o_c

    xs = tuple(jnp.moveaxis(t, 2, 0) for t in (q_dec, k_end, v, chunk_decay))
    s_final, o_inter = lax.scan(step, s0, xs)
    o = o_intra + jnp.moveaxis(o_inter, 0, 2)
    return o.reshape(b, h, length, dv), s_final


def _hgrn2_bidir(pq, pzf, pzb, pv, lb, s0_f, s0_b):
    q = _to_heads(jax.nn.silu(pq.astype(jnp.float32)), HG_DK)
    v = _to_heads(pv.astype(jnp.float32), HG_DV)
    k_f, lf_f = _forget_gate(pzf, lb[0])
    k_b, lf_b = _forget_gate(pzb, lb[1])
    o_f, s_f = _gla_chunked(q, k_f, v, lf_f, s0_f)
    rev = lambda t: jnp.flip(t, axis=2)
    o_b, s_b = _gla_chunked(rev(q), rev(k_b), rev(v), rev(lf_b), s0_b)
    return o_f + rev(o_b), s_f, s_b


def _hgrn2_readout(o, pg, norm_g):
    b, _, length, _ = o.shape
    o = o.transpose(0, 2, 1, 3)
    o = o * lax.rsqrt(jnp.mean(o * o, axis=-1, keepdims=True) + RMS_EPS) * norm_g.astype(jnp.float32).reshape(HG_HEADS, HG_DV)
    y = o.reshape(b, length, HG_WIDTH) * jax.nn.silu(pg.astype(jnp.float32))
    return y.astype(pg.dtype)


def _merge(y_f, y_h, gates, w_fo, w_ho, w_o):
    g_f, g_h = jnp.split(gates, N_BRANCHES, axis=-1)
    m = jax.nn.sigmoid(g_f) * (y_f @ w_fo) + jax.nn.sigmoid(g_h) * (y_h @ w_ho)
    return m @ w_o


def _moe(h, w_router, b_router, w_gu, b_gu, w_down, b_down):
    n_tok, d = h.shape
    logits = (h @ w_router).astype(jnp.float32) + b_router.astype(jnp.float32)
    top_val, top_idx = lax.top_k(logits, TOP_K)
    top_w = jax.nn.softmax(top_val, axis=-1)
    n_asg = n_tok * TOP_K
    e_flat = top_idx.reshape(n_asg)
    order = jnp.argsort(e_flat)
    e_sorted = e_flat[order]
    tok_sorted = (order // TOP_K).astype(jnp.int32)
    w_sorted = top_w.reshape(n_asg)[order]
    counts = jnp.bincount(e_flat, length=N_EXPERTS)
    starts = jnp.cumsum(counts) - counts
    padded = (counts + MOE_BLOCK - 1) // MOE_BLOCK * MOE_BLOCK
    pad_ends = jnp.cumsum(padded)
    dest = pad_ends[e_sorted] - padded[e_sorted] + jnp.arange(n_asg) - starts[e_sorted]
    n_blocks = -(-n_asg // MOE_BLOCK) + N_EXPERTS
    n_rows = n_blocks * MOE_BLOCK
    row_tok = jnp.full((n_rows,), n_tok, dtype=jnp.int32).at[dest].set(tok_sorted)
    row_w = jnp.zeros((n_rows,), jnp.float32).at[dest].set(w_sorted)
    block_expert = jnp.minimum(jnp.searchsorted(pad_ends, jnp.arange(n_blocks) * MOE_BLOCK, side='right'), N_EXPERTS - 1)
    h_pad = jnp.concatenate([h, jnp.zeros((1, d), h.dtype)], axis=0)

    def expert_block(args):
        toks, e = args
        gu = h_pad[toks] @ w_gu[e] + b_gu[e]
        gate, up = jnp.split(gu, 2, axis=-1)
        gate = jnp.minimum(gate, SWIGLU_LIMIT)
        up = jnp.clip(up, -SWIGLU_LIMIT, SWIGLU_LIMIT)
        act = (up + 1.0) * gate * jax.nn.sigmoid(SWIGLU_ALPHA * gate)
        return act @ w_down[e] + b_down[e]

    out = lax.map(expert_block, (row_tok.reshape(n_blocks, MOE_BLOCK), block_expert))
    out = out.reshape(n_rows, d).astype(jnp.float32) * row_w[:, None]
    y = jax.ops.segment_sum(out, row_tok, num_segments=n_tok + 1)[:n_tok]
    return y.astype(h.dtype)


def setup_inputs(seed: int = 0) -> dict:
    key = jax.random.key(seed)
    ks = jax.random.split(key, 24)
    nrm = lambda k, shape, scale: jax.random.normal(k, shape, jnp.float32) * scale
    return {
        'x': nrm(ks[0], (BATCH, SEQ, D_MODEL), 1.0),
        'c': nrm(ks[1], (BATCH, D_MODEL), 1.0),
        'ctx': nrm(ks[2], (BATCH, CTX_LEN, D_MODEL), 1.0),
        'c_ctx': nrm(ks[3], (D_MODEL,), 1.0),
        'w_ada': nrm(ks[4], (DEPTH, D_MODEL, N_MOD * D_MODEL), 0.5 * D_MODEL ** -0.5),
        'b_ada': nrm(ks[5], (DEPTH, N_MOD * D_MODEL), 0.02),
        'w_in': nrm(ks[6], (DEPTH, D_MODEL, IN_WIDTH), D_MODEL ** -0.5),
        'lb_raw': nrm(ks[7], (DEPTH + 1, 2, HG_KEY_WIDTH), 0.1),
        'hg_norm_g': 1.0 + nrm(ks[8], (DEPTH, HG_WIDTH), 0.02),
        'w_four_out': nrm(ks[9], (DEPTH, F_WIDTH, D_MODEL), F_WIDTH ** -0.5),
        'w_hg_out': nrm(ks[10], (DEPTH, HG_WIDTH, D_MODEL), HG_WIDTH ** -0.5),
        'w_o': nrm(ks[11], (DEPTH, D_MODEL, D_MODEL), DEEPNORM_BETA * D_MODEL ** -0.5),
        'ln1_g': 1.0 + nrm(ks[12], (DEPTH, D_MODEL), 0.02),
        'ln1_b': nrm(ks[13], (DEPTH, D_MODEL), 0.02),
        'w_router': nrm(ks[14], (DEPTH, D_MODEL, N_EXPERTS), D_MODEL ** -0.5),
        'b_router': nrm(ks[15], (DEPTH, N_EXPERTS), 0.01),
        'w_gate_up': nrm(ks[16], (DEPTH, N_EXPERTS, D_MODEL, 2 * D_EXPERT), D_MODEL ** -0.5),
        'b_gate_up': nrm(ks[17], (DEPTH, N_EXPERTS, 2 * D_EXPERT), 0.02),
        'w_down': nrm(ks[18], (DEPTH, N_EXPERTS, D_EXPERT, D_MODEL), DEEPNORM_BETA * D_EXPERT ** -0.5),
        'b_down': nrm(ks[19], (DEPTH, N_EXPERTS, D_MODEL), 0.02),
        'ln2_g': 1.0 + nrm(ks[20], (DEPTH, D_MODEL), 0.02),
        'ln2_b': nrm(ks[21], (DEPTH, D_MODEL), 0.02),
    }


def reference(x, c, ctx, c_ctx, w_ada, b_ada, w_in, lb_raw, hg_norm_g, w_four_out, w_hg_out, w_o, ln1_g, ln1_b, w_router, b_router, w_gate_up, b_gate_up, w_down, b_down, ln2_g, ln2_b):
    b = x.shape[0]
    lower_bounds = jnp.cumsum(jax.nn.softmax(lb_raw.astype(jnp.float32), axis=0), axis=0)
    c_act = jax.nn.silu(c)
    c_ctx_act = jax.nn.silu(c_ctx)
    xc = ctx
    for l in range(DEPTH):
        last = l == DEPTH - 1
        shift1, scale1, gate1, shift2, scale2, gate2 = (t[:, None, :] for t in jnp.split(c_act @ w_ada[l] + b_ada[l], N_MOD, axis=-1))
        cshift1, cscale1, cgate1, cshift2, cscale2, cgate2 = jnp.split(c_ctx_act @ w_ada[l] + b_ada[l], N_MOD, axis=-1)
        lb = lower_bounds[l]
        u = x * (1.0 + scale1) + shift1
        uc = xc * (1.0 + cscale1) + cshift1
        pf, pq, pzf, pzb, pv, pg, gates = _split_in(u @ w_in[l])
        cpf, cpq, cpzf, cpzb, cpv, cpg, cgates = _split_in(uc @ w_in[l])
        zero_state = jnp.zeros((b, HG_HEADS, HG_DK, HG_DV), jnp.float32)
        oc, s_f, s_b = _hgrn2_bidir(cpq, cpzf, cpzb, cpv, lb, zero_state, zero_state)
        o, _, _ = _hgrn2_bidir(pq, pzf, pzb, pv, lb, s_f, s_b)
        mix = _merge(_fourier_latent(pf), _hgrn2_readout(o, pg, hg_norm_g[l]), gates, w_four_out[l], w_hg_out[l], w_o[l])
        x = _layer_norm(DEEPNORM_ALPHA * x + gate1 * mix, ln1_g[l], ln1_b[l])
        u2 = x * (1.0 + scale2) + shift2
        ff = _moe(u2.reshape(-1, D_MODEL), w_router[l], b_router[l], w_gate_up[l], b_gate_up[l], w_down[l], b_down[l]).reshape(x.shape)
        x = _layer_norm(DEEPNORM_ALPHA * x + gate2 * ff, ln2_g[l], ln2_b[l])
        if not last:
            mix_c = _merge(_fourier_context(cpf), _hgrn2_readout(oc, cpg, hg_norm_g[l]), cgates, w_four_out[l], w_hg_out[l], w_o[l])
            xc = _layer_norm(DEEPNORM_ALPHA * xc + cgate1 * mix_c, ln1_g[l], ln1_b[l])
            u2c = xc * (1.0 + cscale2) + cshift2
            ffc = _moe(u2c.reshape(-1, D_MODEL), w_router[l], b_router[l], w_gate_up[l], b_gate_up[l], w_down[l], b_down[l]).reshape(xc.shape)
            xc = _layer_norm(DEEPNORM_ALPHA * xc + cgate2 * ffc, ln2_g[l], ln2_b[l])
    return x
```

```python
import numpy as np
import ml_dtypes
import concourse.bass as bass
import concourse.mybir as mybir
from contextlib import ExitStack
from concourse.bass_utils import run_bass_kernel_spmd

F32 = mybir.dt.float32
BF16 = mybir.dt.bfloat16
I32 = mybir.dt.int32
AF = mybir.ActivationFunctionType
ALU = mybir.AluOpType
AX = mybir.AxisListType

NCORES = 8
NB = 4
L = 2048
LC = 256
D = 1024
NT = L // 128
NTOK = NB * L
NTT = NTOK // 128
NE = 32
BLK = 512
NBLK = NTOK * 4 // BLK + NE
NROWS = NBLK * BLK
ALPHA = 2.0 ** 0.25
LN_EPS = 1e-5
RMS_EPS = 1e-6


class R:
    __slots__ = ("name", "w", "r")

    def __init__(self, name=""):
        self.name = name
        self.w = {}
        self.r = {}


class Prog:
    ENG = ("pe", "act", "dve", "pool", "sp")

    def __init__(self, nc, es):
        self.nc = nc
        self.es = es
        self.q = {e: [] for e in self.ENG}
        self.semh = {}
        self.cnt = {}
        self.seen = {e: {} for e in self.ENG}
        for e in self.ENG:
            self._sem("c_" + e)

    def _sem(self, name):
        if name not in self.semh:
            self.semh[name] = self.es.enter_context(self.nc.semaphore(name))
            self.cnt[name] = 0
        return self.semh[name]

    def op(self, eng, fn, reads=(), writes=(), ch=None, wacc=()):
        waits = {}

        def merge(d):
            for s, v in d.items():
                if waits.get(s, 0) < v:
                    waits[s] = v
        for r in reads:
            merge(r.w)
        for r in writes:
            merge(r.w)
            merge(r.r)
        for r in wacc:
            merge(r.r)
        own = "c_" + eng
        seen = self.seen[eng]
        wl = []
        for s, v in waits.items():
            if s == own and eng == "pe":
                continue
            if seen.get(s, 0) >= v:
                continue
            seen[s] = v
            wl.append((s, v))
        if ch is None:
            sname, inc = own, 1
        else:
            sname, inc = "d_" + ch, 16
            self._sem(sname)
        self.cnt[sname] += inc
        val = self.cnt[sname]
        self.q[eng].append((wl, fn, sname, inc))
        for r in reads:
            if r.r.get(sname, 0) < val:
                r.r[sname] = val
        for r in writes:
            r.w = {sname: val}
            r.r = {}
        for r in wacc:
            r.w[sname] = val

    def fence(self):
        snap = dict(self.cnt)
        for eng in self.ENG:
            wl = []
            seen = self.seen[eng]
            for s, v in snap.items():
                if v == 0 or seen.get(s, 0) >= v:
                    continue
                if s == "c_" + eng or s in ("d_cast", "d_dbg", "d_zf"):
                    continue
                seen[s] = v
                wl.append((s, v))
            if wl:
                self.q[eng].append((wl, None, None, 0))

    def final_wait(self, eng, resources):
        waits = {}
        for r in resources:
            for s, v in list(r.w.items()) + list(r.r.items()):
                if waits.get(s, 0) < v:
                    waits[s] = v
        self.q[eng].append((list(waits.items()), None, None, 0))

    def emit(self):
        nc = self.nc
        with nc.Block() as block:
            def replay(name, engobj):
                for wl, fn, sname, inc in self.q[name]:
                    for s, v in wl:
                        engobj.wait_ge(self.semh[s], v)
                    if fn is not None:
                        fn(engobj).then_inc(self.semh[sname], inc)

            @block.tensor
            def _(e):
                replay("pe", e)

            @block.scalar
            def _(e):
                replay("act", e)

            @block.vector
            def _(e):
                replay("dve", e)

            @block.gpsimd
            def _(e):
                replay("pool", e)

            @block.sync
            def _(e):
                replay("sp", e)


def _consts():
    c = {}
    c["ident_f"] = np.eye(128, dtype=np.float32)
    c["ident_b"] = np.eye(128, dtype=np.float32).astype(ml_dtypes.bfloat16)
    t = np.arange(128)
    same = (t[:, None] // 64) == (t[None, :] // 64)
    inc_f = (same & (t[:, None] <= t[None, :])).astype(np.float32)
    su_f = (same & (t[:, None] > t[None, :])).astype(np.float32)
    c["tri"] = np.stack([np.stack([inc_f, su_f, -inc_f]), np.stack([inc_f.T, su_f.T, -inc_f.T])]).astype(np.float32)
    c["tri"] = np.ascontiguousarray(c["tri"].transpose(2, 0, 1, 3)).astype(ml_dtypes.bfloat16)
    c["ones_f"] = np.ones((128, 128), np.float32)
    tt = np.arange(L)
    r, w = tt // 64, tt % 64
    ph = (np.outer(r, r) / 32.0 + np.outer(w, w) / 64.0) * 2 * np.pi
    c["ctok"] = (np.cos(ph) / np.sqrt(L)).astype(ml_dtypes.bfloat16)
    c["stok"] = (np.sin(ph) / np.sqrt(L)).astype(ml_dtypes.bfloat16)
    cc = np.arange(128)
    phc = np.outer(cc, cc) * 2 * np.pi / 128.0
    c["cch"] = (np.cos(phc) / np.sqrt(128)).astype(ml_dtypes.bfloat16)
    c["schn"] = (-np.sin(phc) / np.sqrt(128)).astype(ml_dtypes.bfloat16)
    c["ltri"] = (t[:, None] < t[None, :]).astype(np.float32)
    ee = np.arange(NE)
    c["le_mask"] = np.tile((ee[None, :] <= ee[:, None]).astype(np.float32).reshape(1, NE * NE), (128, 1))
    c["ones_b"] = np.ones((128, 128), np.float32).astype(ml_dtypes.bfloat16)
    c["iota_e"] = np.tile(np.arange(NE, dtype=np.float32)[None, :], (128, 1))
    c["iota_p"] = np.arange(128, dtype=np.float32)[:, None].copy()
    c["blk_thr"] = np.tile((np.arange(NBLK, dtype=np.float32) * BLK)[None, :], (128, 1))
    return c


CONST_DT = {"ident_f": F32, "ident_b": BF16, "tri": BF16, "ones_f": F32, "ctok": BF16, "stok": BF16,
            "cch": BF16, "schn": BF16, "ltri": F32, "le_mask": F32, "ones_b": BF16, "iota_e": F32, "iota_p": F32, "blk_thr": F32}


def build(debug=False):
    nc = bass.Bass("TRN2", target_bir_lowering=False)
    consts = _consts()

    def din(name, shape, dt=F32):
        return nc.dram_tensor(name, list(shape), dt, kind="ExternalInput").ap()

    def dscr(name, shape, dt):
        return nc.dram_tensor(name, list(shape), dt, kind="Internal").ap()

    x = din("x", [NB, L, D])
    ctx = din("ctx", [NB, LC, D])
    c5 = din("c5", [128, 8, 5])
    w_ada = din("w_ada", [D, 6 * D])
    b_adaT = din("b_adaT", [128, 48])
    b_ada = din("b_ada", [1, 6 * D])
    w_in = din("w_in", [D, 5120])
    lb_raw = din("lb_raw", [2, 2, 512])
    normgT = din("normgT", [128, 4])
    w_fo = din("w_four_out", [512, D])
    w_ho = din("w_hg_out", [512, D])
    w_o = din("w_o", [D, D])
    ln1_g = din("ln1_g", [1, D])
    ln1_b = din("ln1_b", [1, D])
    w_router = din("w_router", [D, NE])
    b_router = din("b_router", [1, NE])
    w_gu = din("w_gate_up", [NE, D, 2 * D])
    bguT = din("b_guT", [NE * 128, 16])
    w_dn = din("w_down", [NE, D, D])
    b_dn = din("b_down", [NE, D])
    ln2_g = din("ln2_g", [1, D])
    ln2_b = din("ln2_b", [1, D])
    cd = {k: din("k_" + k, v.shape, CONST_DT[k]) for k, v in consts.items()}
    out = nc.dram_tensor("out", [NB, L, D], F32, kind="ExternalOutput").ap()

    winb = dscr("winb", [D, 5120], BF16)
    wfob = dscr("wfob", [512, D], BF16)
    whob = dscr("whob", [512, D], BF16)
    wob = dscr("wob", [D, D], BF16)
    wgub = dscr("wgub", [NE * 128, 8 * 2 * D], BF16)
    wdnb = dscr("wdnb", [NE * 128, 8 * D], BF16)
    modrows = dscr("modrows", [5, 6 * D], F32)
    x1s = dscr("x1s", [NTOK, D], F32)
    u2b = dscr("u2b", [NTOK, D], BF16)
    xs = dscr("xs", [NROWS, D], BF16)
    ys = dscr("ys", [NROWS, D], F32)
    dbg = {}
    if debug:
        dbg["x1"] = nc.dram_tensor("dbg_x1", [NTOK, D], F32, kind="ExternalOutput").ap()
        dbg["yh"] = nc.dram_tensor("dbg_yh", [128, 4, L], F32, kind="ExternalOutput").ap()
        dbg["yf"] = nc.dram_tensor("dbg_yf", [128, 4, L], F32, kind="ExternalOutput").ap()
        dbg["mod"] = nc.dram_tensor("dbg_mod", [128, 48, 5], F32, kind="ExternalOutput").ap()

    with ExitStack() as es:
        p = Prog(nc, es)

        uniq = [0]

        def sb(scope, name, shape, dt=F32):
            uniq[0] += 1
            return scope.enter_context(nc.sbuf_tensor(f"{name}_{uniq[0]}", list(shape), dt))

        ps = [es.enter_context(nc.psum_tensor(f"ps{i}", [128, 512], F32)) for i in range(8)]
        psr = [R(f"ps{i}") for i in range(8)]
        bank_i = [0]

        bank_skip = [None]

        def bank():
            i = bank_i[0]
            if i == bank_skip[0]:
                i = (i + 1) % 8
            bank_i[0] = (i + 1) % 8
            return ps[i], psr[i]

        def mm(groups, reads, writes):
            def fn(e):
                inst = None
                for out_ap, pairs in groups:
                    n = len(pairs)
                    for i, (l, r_) in enumerate(pairs):
                        inst = e.matmul(out_ap, l, r_, start=(i == 0), stop=(i == n - 1))
                return inst
            p.op("pe", fn, reads=reads, writes=writes)

        def tr(out_ap, in_ap, ident, reads, writes):
            p.op("pe", lambda e: e.transpose(out_ap, in_ap, ident), reads=reads, writes=writes)

        def act(out_ap, in_ap, func, reads, writes, bias=None, scale=None):
            kw = {}
            if bias is not None:
                kw["bias"] = bias
            if scale is not None:
                kw["scale"] = scale
            p.op("act", lambda e: e.activation(out_ap, in_ap, func, **kw), reads=reads, writes=writes)

        def tt(eng, out_ap, a, b, op, reads, writes):
            p.op(eng, lambda e: e.tensor_tensor(out_ap, a, b, op), reads=reads, writes=writes)

        def ts(eng, out_ap, a, s1, s2, op0, op1, reads, writes):
            if op1 is None:
                p.op(eng, lambda e: e.tensor_scalar(out_ap, a, s1, None, op0), reads=reads, writes=writes)
            else:
                p.op(eng, lambda e: e.tensor_scalar(out_ap, a, s1, s2, op0, op1), reads=reads, writes=writes)

        def stt(out_ap, a, s, b, op0, op1, reads, writes):
            p.op("dve", lambda e: e.scalar_tensor_tensor(out_ap, a, s, b, op0, op1), reads=reads, writes=writes)

        def cp(eng, out_ap, in_ap, reads, writes):
            if eng == "act":
                p.op("act", lambda e: e.copy(out_ap, in_ap), reads=reads, writes=writes)
            else:
                p.op(eng, lambda e: e.tensor_copy(out_ap, in_ap), reads=reads, writes=writes)

        def dma(eng, out_ap, in_ap, reads, writes, ch, wacc=(), **kw):
            p.op(eng, lambda e: e.dma_start(out=out_ap, in_=in_ap, **kw), reads=reads, writes=writes, ch=ch, wacc=wacc)

        def dbgout(name, ap, shape, dt, reads):
            if not debug:
                return
            t = nc.dram_tensor("dbg_" + name, list(shape), dt, kind="ExternalOutput").ap()
            dma("pool", t, ap, reads, [R()], ch="dbg")

        k = {}
        kr = {}
        for name, arr in consts.items():
            if name in ("ctok", "stok", "le_mask"):
                continue
            k[name] = sb(es, "c_" + name, arr.shape, CONST_DT[name])
            kr[name] = R(name)
            dma("sp", k[name][:], cd[name], [], [kr[name]], ch="const")
        KR = list(kr.values())

        r_winb, r_wfob, r_whob, r_wob, r_wgub, r_wdnb = [R() for _ in range(6)]
        for j in range(10):
            dma("pool", winb[:, j * 512:(j + 1) * 512], w_in[:, j * 512:(j + 1) * 512], [], [], ch="cast_in", wacc=[r_winb])
        dma("pool", wfob, w_fo, [], [r_wfob], ch="cast_fo")
        dma("pool", whob, w_ho, [], [r_whob], ch="cast_ho")
        dma("pool", wob, w_o, [], [r_wob], ch="cast_o")
        zt = sb(es, "zt", [128, D], BF16)
        r_zt = R()
        r_zf = R()
        p.op("dve", lambda e: e.memset(zt[:], 0.0), writes=[r_zt])
        ZR = 4096
        for c_ in range(NROWS // ZR):
            dma("pool", xs[c_ * ZR:(c_ + 1) * ZR, :].rearrange("(n p) d -> p n d", p=128),
                zt[:].unsqueeze(1).to_broadcast([128, ZR // 128, D]), [r_zt], [], ch="zf", wacc=[r_zf])
        modT = sb(es, "modT", [128, 48, 5])
        r_modT = R()
        r_modrows = R()
        lbt = sb(es, "lbt", [128, 2, 512])
        r_lbt = R()
        normg = sb(es, "normg", [128, 4])
        r_normg = R()
        dma("sp", normg[:], normgT, [], [r_normg], ch="const")
        wr = sb(es, "wr", [128, 8, NE])
        r_wr = R()
        dma("sp", wr[:], w_router.rearrange("(kc p) e -> p kc e", p=128), [], [r_wr], ch="const")
        brt = sb(es, "brt", [128, NE])
        r_brt = R()
        dma("sp", brt[:], b_router.partition_broadcast(128), [], [r_brt], ch="const")
        for r_ in KR + [r_normg, r_wr, r_brt]:
            r_.w = {"d_const": p.cnt["d_const"]}
        epst = sb(es, "epst", [128, 3])
        r_eps = R()
        p.op("dve", lambda e: e.memset(epst[:, 0:1], RMS_EPS), writes=[r_eps])
        p.op("dve", lambda e: e.memset(epst[:, 1:2], LN_EPS), writes=[r_eps])
        p.op("dve", lambda e: e.memset(epst[:, 2:3], 1.0), writes=[r_eps])
        r_x1s, r_u2b, r_xs, r_ys = R(), R(), R(), R()
        Ltab = sb(es, "Ltab", [128, NTT, NE])
        Vtab = sb(es, "Vtab", [128, NTT, 8])
        r_tab = [R() for _ in range(NTT)]

        with ExitStack() as s0:
            cact = sb(s0, "cact", [128, 8, 5])
            r_cact = R()
            dma("sp", cact[:], c5, [], [r_cact], ch="p0a")
            badaT = sb(s0, "badaT", [128, 48])
            r_badaT = R()
            dma("sp", badaT[:], b_adaT, [], [r_badaT], ch="p0a")
            bada5 = sb(s0, "bada5", [5, 6 * D])
            r_bada5 = R()
            dma("sp", bada5[:], b_ada.partition_broadcast(5), [], [r_bada5], ch="p0a")
            lraw = sb(s0, "lraw", [128, 2, 2, 512])
            r_lraw = R()
            dma("sp", lraw[:], lb_raw.partition_broadcast(128), [], [r_lraw], ch="p0a")
            for r_ in (r_cact, r_badaT, r_bada5, r_lraw):
                r_.w = {"d_p0a": p.cnt["d_p0a"]}
            act(cact[:], cact[:], AF.Silu, [r_cact], [r_cact])
            rows = sb(s0, "rows", [5, 6 * D])
            r_rows = R()
            wa = [sb(s0, f"wa{i}", [128, 8, 512]) for i in range(2)]
            r_wa = [R(), R()]
            pm, r_pm = bank()
            bank_skip[0] = 0
            for g in range(12):
                i = g % 2
                dma("sp", wa[i][:], w_ada[:, g * 512:(g + 1) * 512].rearrange("(kc p) n -> p kc n", p=128),
                    [], [r_wa[i]], ch=f"wa{i}")
                groups = []
                for jj in range(4):
                    j = g * 4 + jj
                    groups.append((pm[:, j * 5:(j + 1) * 5],
                                   [(wa[i][:, kc, jj * 128:(jj + 1) * 128], cact[:, kc, :]) for kc in range(8)]))
                mm(groups, [r_wa[i], r_cact], [r_pm])
                pr, r_pr = bank()
                mm([(pr[0:5, :], [(cact[:, kc, :], wa[i][:, kc, :]) for kc in range(8)])], [r_wa[i], r_cact], [r_pr])
                tt("dve", rows[:, g * 512:(g + 1) * 512], pr[0:5, :], bada5[:, g * 512:(g + 1) * 512], ALU.add,
                   [r_pr, r_bada5], [r_rows])
            tt("dve", modT[:], pm[:, 0:240].rearrange("p (j b) -> p j b", b=5),
               badaT[:].unsqueeze(2).to_broadcast([128, 48, 5]), ALU.add, [r_pm, r_badaT], [r_modT])
            for j0 in (8, 32):
                ts("dve", modT[:, j0:j0 + 8, :], modT[:, j0:j0 + 8, :], 1.0, None, ALU.add, None, [r_modT], [r_modT])
                ts("dve", rows[:, j0 * 128:(j0 + 8) * 128], rows[:, j0 * 128:(j0 + 8) * 128], 1.0, None, ALU.add, None,
                   [r_rows], [r_rows])
            bank_skip[0] = None
            dma("sp", modrows, rows[:], [r_rows], [r_modrows], ch="p0s")
            if debug:
                dma("pool", dbg["mod"], modT[:], [r_modT], [R()], ch="dbg")
            ldiff = sb(s0, "ldiff", [128, 2, 512])
            r_ldiff = R()
            tt("dve", ldiff[:], lraw[:, 0, :, :], lraw[:, 1, :, :], ALU.subtract, [r_lraw], [r_ldiff])
            for dr in range(2):
                act(lbt[:, dr, :], ldiff[:, dr, :], AF.Sigmoid, [r_ldiff], [r_lbt], scale=-1.0)
                act(lbt[:, dr, :], lbt[:, dr, :], AF.Ln, [r_lbt], [r_lbt])

        p.fence()
        pending_casts = []
        for e_ in range(NE):
            pending_casts.append((wgub[e_ * 128:(e_ + 1) * 128, :].rearrange("p (kc f) -> p kc f", kc=8),
                                  w_gu[e_].rearrange("(kc p) f -> p kc f", p=128), r_wgub))
            pending_casts.append((wdnb[e_ * 128:(e_ + 1) * 128, :].rearrange("p (kc f) -> p kc f", kc=8),
                                  w_dn[e_].rearrange("(kc p) f -> p kc f", p=128), r_wdnb))

        def issue_casts(n):
            for _ in range(min(n, len(pending_casts))):
                o_, i_, r_ = pending_casts.pop(0)
                dma("pool", o_, i_, [], [], ch="cast", wacc=[r_])

        with ExitStack() as sm:
            uT = sb(sm, "uT", [128, 8, L], BF16)
            r_uT = R()
            ucT = sb(sm, "ucT", [128, 8, LC], BF16)
            r_ucT = R()
            wg = [sb(sm, f"wg{i}", [128, 8, 512], BF16) for i in range(2)]
            r_wg = [R(), R()]
            wg_i = [0]
            yfT = sb(sm, "yfT", [128, 4, L], BF16)
            r_yfT = R()
            yhT = sb(sm, "yhT", [128, 4, L], BF16)
            r_yhT = R()

            def load_wg(c0, ncols=512):
                i = wg_i[0]
                wg_i[0] = 1 - i
                dma("sp", wg[i][:, :, 0:ncols], winb[:, c0:c0 + ncols].rearrange("(kc p) n -> p kc n", p=128),
                    [r_winb], [r_wg[i]], ch=f"wg{i}")
                return wg[i], r_wg[i]

            for b in range(NB):
                p.fence()
                with ExitStack() as s1:
                    xt = [sb(s1, f"xt{i}", [128, 4, D]) for i in range(2)]
                    r_xt = [R(), R()]

                    def make_uT(src, ntiles, dst, r_dst, col):
                        ngr = (ntiles + 3) // 4
                        for g in range(ngr):
                            i = g % 2
                            nt = min(4, ntiles - g * 4)
                            dma("sp", xt[i][:, 0:nt, :],
                                src[g * 512:g * 512 + nt * 128, :].rearrange("(a p) d -> p a d", p=128),
                                [], [r_xt[i]], ch=f"xt{i}")
                            for kc in range(8):
                                pb, r_pb = bank()
                                for a in range(nt):
                                    tr(pb[:, a * 128:(a + 1) * 128], xt[i][:, a, kc * 128:(kc + 1) * 128], k["ident_f"][:],
                                       [r_xt[i], kr["ident_f"]], [r_pb])
                                act(dst[:, kc, g * 512:g * 512 + nt * 128], pb[:, 0:nt * 128], AF.Identity,
                                    [r_pb, r_modT], [r_dst], bias=modT[:, kc, col:col + 1], scale=modT[:, 8 + kc, col:col + 1])
                    make_uT(x[b], NT, uT, r_uT, b)
                    make_uT(ctx[b], 2, ucT, r_ucT, 4)
                    if b == 0:
                        dbgout("uT", uT[:], [128, 8, L], BF16, [r_uT])

                p.fence()
                with ExitStack() as s3:
                    qT = sb(s3, "qT", [128, 4, L], BF16)
                    r_qT = R()
                    vtm = sb(s3, "vtm", [128, NT, 512], BF16)
                    r_vtm = R()
                    vctm = sb(s3, "vctm", [128, 2, 512], BF16)
                    r_vctm = R()
                    oacc = sb(s3, "oacc", [128, 4, L], BF16)
                    r_oacc = [R() for _ in range(NT)]
                    LF32 = sb(s3, "LF32", [128, 512])
                    LK32 = sb(s3, "LK32", [128, 512])
                    r_LF32, r_LK32 = R(), R()
                    LFh = [sb(s3, f"LFh{d_}", [128, 512], BF16) for d_ in range(2)]
                    LFl = [sb(s3, f"LFl{d_}", [128, 512], BF16) for d_ in range(2)]
                    kb = [sb(s3, f"kb{d_}", [128, 512], BF16) for d_ in range(2)]
                    eqn = [sb(s3, f"eqn{d_}", [128, 512]) for d_ in range(2)]
                    er = sb(s3, "er", [128, 512], BF16)
                    r_eqn = [R(), R()]
                    r_er = R()
                    r_LF = [R(), R()]
                    r_LK = [R(), R()]
                    sg = sb(s3, "sg", [128, 512])
                    sgn = sb(s3, "sgn", [128, 512])
                    r_sg, r_sgn = R(), R()
                    eq = [sb(s3, f"eq{d_}", [128, 512]) for d_ in range(2)]
                    r_eq = [R(), R()]
                    qdec = [sb(s3, f"qdec{d_}", [128, 4, 128], BF16) for d_ in range(2)]
                    kinv = [sb(s3, f"kinv{d_}", [128, 4, 128], BF16) for d_ in range(2)]
                    kend = [sb(s3, f"kend{d_}", [128, 512], BF16) for d_ in range(2)]
                    scm = [sb(s3, f"scm{d_}", [128, 4, 128], BF16) for d_ in range(2)]
                    r_qdec, r_kinv, r_kend, r_scm = [[R(), R()] for _ in range(4)]
                    S = [sb(s3, f"S{d_}", [128, 4, 128]) for d_ in range(2)]
                    Sb = [sb(s3, f"Sb{d_}", [128, 4, 4, 128], BF16) for d_ in range(2)]
                    r_S = [R(), R()]
                    r_Sb = [[R() for _ in range(4)] for _ in range(2)]

                    wq, r_wq = load_wg(512)
                    for tq in range(4):
                        for h in range(4):
                            pb, r_pb = bank()
                            mm([(pb[:], [(wq[:, kc, h * 128:(h + 1) * 128], uT[:, kc, tq * 512:(tq + 1) * 512]) for kc in range(8)])],
                               [r_wq, r_uT], [r_pb])
                            act(qT[:, h, tq * 512:(tq + 1) * 512], pb[:], AF.Silu, [r_pb], [r_qT])
                    wv, r_wv = load_wg(2048)
                    for ti in range(NT):
                        pb, r_pb = bank()
                        mm([(pb[:], [(uT[:, kc, ti * 128:(ti + 1) * 128], wv[:, kc, :]) for kc in range(8)])], [r_wv, r_uT], [r_pb])
                        cp("act", vtm[:, ti, :], pb[:], [r_pb], [r_vtm])
                    for ti in range(2):
                        pb, r_pb = bank()
                        mm([(pb[:], [(ucT[:, kc, ti * 128:(ti + 1) * 128], wv[:, kc, :]) for kc in range(8)])], [r_wv, r_ucT], [r_pb])
                        cp("act", vctm[:, ti, :], pb[:], [r_pb], [r_vctm])
                    wz = [None, None]
                    r_wz = [None, None]
                    wz[0], r_wz[0] = load_wg(1024)
                    wz[1], r_wz[1] = load_wg(1536)

                    for dr in range(2):
                        p.op("dve", lambda e, dr=dr, S=S: e.memset(S[dr][:], 0.0), writes=[r_S[dr]])
                        p.op("dve", lambda e, dr=dr, Sb=Sb: e.memset(Sb[dr][:], 0.0), writes=r_Sb[dr])

                    def gla_step(dr, step, ti, srcT, r_src, vt, r_vt, latent, first_write):
                        tri = k["tri"]
                        issue_casts(1)
                        pz, r_pz = bank()
                        mm([(pz[:], [(srcT[:, kc, ti * 128:(ti + 1) * 128], wz[dr][:, kc, :]) for kc in range(8)])],
                           [r_wz[dr], r_src], [r_pz])
                        act(sg[:], pz[:], AF.Exp, [r_pz], [r_sg])
                        act(sgn[:], sg[:], AF.Ln, [r_sg, r_eps], [r_sgn], bias=epst[:, 2:3], scale=1.0)
                        tt("dve", LK32[:], lbt[:, dr, :], sgn[:], ALU.subtract, [r_sgn, r_lbt], [r_LK32])
                        act(sg[:], LK32[:], AF.Exp, [r_LK32], [r_sg])
                        act(LF32[:], sg[:], AF.Ln, [r_sg, r_eps], [r_LF32], bias=epst[:, 2:3], scale=-1.0)
                        cp("act", LFh[dr][:], LF32[:], [r_LF32], [r_LF[dr]])
                        tt("dve", LFl[dr][:], LF32[:], LFh[dr][:], ALU.subtract, [r_LF32, r_LF[dr]], [r_LF[dr]])
                        cp("dve", kb[dr][:], sg[:], [r_sg], [r_LK[dr]])
                        pq, r_pq = bank()
                        mm([(pq[:, h * 128:(h + 1) * 128], [(LFh[dr][:, h * 128:(h + 1) * 128], tri[:, dr, 0, :]),
                                                           (LFl[dr][:, h * 128:(h + 1) * 128], tri[:, dr, 0, :])]) for h in range(4)],
                           [r_LF[dr], kr["tri"]], [r_pq])
                        act(eq[dr][:], pq[:], AF.Exp, [r_pq], [r_eq[dr]])
                        if latent:
                            act(eqn[dr][:], pq[:], AF.Exp, [r_pq], [r_eqn[dr]], scale=-1.0)
                        pe_, r_pe = bank()
                        mm([(pe_[:], [(tri[:, dr, 1, :], LFh[dr][:]), (tri[:, dr, 1, :], LFl[dr][:])])],
                           [r_LF[dr], kr["tri"]], [r_pe])
                        act(er[:], pe_[:], AF.Exp, [r_pe], [r_er])
                        tt("dve", kend[dr][:], er[:], kb[dr][:], ALU.mult, [r_er, r_LK[dr]], [r_kend[dr]])
                        if latent:
                            pk, r_pk = bank()
                            mm([(pk[:, h * 128:(h + 1) * 128], [(kb[dr][:, h * 128:(h + 1) * 128], k["ident_b"][:])]) for h in range(4)],
                               [r_LK[dr], kr["ident_b"]], [r_pk])
                            tt("dve", kinv[dr][:].rearrange("p h t -> p (h t)"), pk[:], eqn[dr][:], ALU.mult, [r_pk, r_eqn[dr]], [r_kinv[dr]])
                            tt("dve", qdec[dr][:], eq[dr][:].rearrange("p (h t) -> p h t", h=4), qT[:, :, ti * 128:(ti + 1) * 128], ALU.mult,
                               [r_eq[dr], r_qT], [r_qdec[dr]])
                            psc, r_psc = bank()
                            mm([(psc[:, h * 128:(h + 1) * 128], [(kinv[dr][:, h, :], qdec[dr][:, h, :])]) for h in range(4)],
                               [r_kinv[dr], r_qdec[dr]], [r_psc])
                            tt("dve", scm[dr][:], psc[:].rearrange("p (h t) -> p h t", h=4),
                               tri[:, dr, 0, :].unsqueeze(1).to_broadcast([128, 4, 128]), ALU.mult, [r_psc, kr["tri"]], [r_scm[dr]])
                        chunks = (0, 1) if dr == 0 else (1, 0)
                        sl = (step % 2) * 2
                        nsl = ((step + 1) % 2) * 2
                        for ci, ch_ in enumerate(chunks):
                            c0 = ch_ * 64
                            gcol = c0 + 63 if dr == 0 else c0
                            pkv, r_pkv = bank()
                            mm([(pkv[:, h * 128:(h + 1) * 128], [(kend[dr][c0:c0 + 64, h * 128:(h + 1) * 128], vt[c0:c0 + 64, ti, h * 128:(h + 1) * 128])])
                                for h in range(4)], [r_kend[dr], r_vt], [r_pkv])
                            for h in range(4):
                                stt(S[dr][:, h, :], S[dr][:, h, :], eq[dr][:, h * 128 + gcol:h * 128 + gcol + 1], pkv[:, h * 128:(h + 1) * 128],
                                    ALU.mult, ALU.add, [r_S[dr], r_eq[dr], r_pkv], [r_S[dr]])
                            dst = sl + 1 if ci == 0 else nsl
                            cp("act", Sb[dr][:, dst, :, :], S[dr][:], [r_S[dr]], [r_Sb[dr][dst]])
                        if latent:
                            po, r_po = bank()

                            def fn(e, dr=dr, ti=ti, chunks=chunks, sl=sl, po=po, vt=vt, scm=scm, Sb=Sb, qdec=qdec):
                                inst = None
                                for h in range(4):
                                    inst = e.matmul(po[:, h * 128:(h + 1) * 128], vt[:, ti, h * 128:(h + 1) * 128], scm[dr][:, h, :],
                                                    start=True, stop=False)
                                    for ci, ch_ in enumerate(chunks):
                                        c0 = ch_ * 64
                                        inst = e.matmul(po[:, h * 128 + c0:h * 128 + c0 + 64], Sb[dr][:, sl + ci, h, :],
                                                        qdec[dr][:, h, c0:c0 + 64], start=False, stop=(ci == 1))
                                return inst
                            p.op("pe", fn, reads=[r_vt, r_scm[dr], r_qdec[dr], r_Sb[dr][sl], r_Sb[dr][sl + 1]], writes=[r_po])
                            ov = oacc[:, :, ti * 128:(ti + 1) * 128]
                            pov = po[:].rearrange("p (h t) -> p h t", h=4)
                            if first_write:
                                cp("act", ov, pov, [r_po], [r_oacc[ti]])
                            else:
                                tt("dve", ov, pov, ov, ALU.add, [r_po, r_oacc[ti]], [r_oacc[ti]])

                    for i in range(2):
                        gla_step(0, i, i, ucT, r_ucT, vctm, r_vctm, False, False)
                        gla_step(1, i, 1 - i, ucT, r_ucT, vctm, r_vctm, False, False)
                    for i in range(NT):
                        gla_step(0, i + 2, i, uT, r_uT, vtm, r_vtm, True, i < NT // 2)
                        gla_step(1, i + 2, NT - 1 - i, uT, r_uT, vtm, r_vtm, True, i < NT // 2)

                    if b == 0:
                        dbgout("qT", qT[:], [128, 4, L], BF16, [r_qT])
                        dbgout("oacc", oacc[:], [128, 4, L], BF16, r_oacc)
                        dbgout("Sf", S[0][:], [128, 4, 128], F32, [r_S[0]])
                    wgg, r_wgg = load_wg(2560)
                    sq = sb(s3, "sq", [128, 512])
                    r_sq = R()
                    rstd = sb(s3, "rstd", [128, 512])
                    r_rstd = R()
                    sgate = sb(s3, "sgate", [128, 512])
                    r_sgate = R()
                    r_ALL_o = r_oacc
                    for tq in range(4):
                        for h in range(4):
                            osl = oacc[:, h, tq * 512:(tq + 1) * 512]
                            act(sq[:], osl, AF.Square, r_ALL_o[tq * 4:(tq + 1) * 4], [r_sq])
                            pb, r_pb = bank()
                            mm([(pb[:], [(k["ones_f"][:], sq[:])])], [r_sq, kr["ones_f"]], [r_pb])
                            act(rstd[:], pb[:], AF.Sqrt, [r_pb, r_eps], [r_rstd], bias=epst[:, 0:1], scale=1.0 / 128.0)
                            p.op("dve", lambda e, rstd=rstd: e.reciprocal(rstd[:], rstd[:]), reads=[r_rstd], writes=[r_rstd])
                            pg_, r_pg = bank()
                            mm([(pg_[:], [(wgg[:, kc, h * 128:(h + 1) * 128], uT[:, kc, tq * 512:(tq + 1) * 512]) for kc in range(8)])],
                               [r_wgg, r_uT], [r_pg])
                            act(sgate[:], pg_[:], AF.Silu, [r_pg], [r_sgate])
                            tt("dve", rstd[:], rstd[:], osl, ALU.mult, [r_rstd] + r_ALL_o[tq * 4:(tq + 1) * 4], [r_rstd])
                            stt(yhT[:, h, tq * 512:(tq + 1) * 512], rstd[:], normg[:, h:h + 1], sgate[:], ALU.mult, ALU.mult,
                                [r_rstd, r_normg, r_sgate], [r_yhT])

                if b == 0:
                    dbgout("yhT", yhT[:], [128, 4, L], BF16, [r_yhT])
                p.fence()
                with ExitStack() as s2:
                    pftm = sb(s2, "pftm", [128, NT, 512], BF16)
                    r_pftm = R()
                    dft = [sb(s2, f"dft{i}", [128, NT, 512], BF16) for i in range(2)]
                    r_dft = [R(), R()]
                    AB = sb(s2, "AB", [128, 2, 4, 512], BF16)
                    r_AB = [R(), R()]
                    wf, r_wf = load_wg(0)
                    for ti in range(NT):
                        pb, r_pb = bank()
                        mm([(pb[:], [(uT[:, kc, ti * 128:(ti + 1) * 128], wf[:, kc, :]) for kc in range(8)])], [r_wf, r_uT], [r_pb])
                        cp("act" if ti % 2 else "dve", pftm[:, ti, :], pb[:], [r_pb], [r_pftm])
                    srcs = (cd["ctok"], cd["stok"])
                    for tq in range(4):
                        for cs in range(2):
                            dma("sp", dft[cs][:], srcs[cs][:, tq * 512:(tq + 1) * 512].rearrange("(ti p) t -> p ti t", p=128),
                                [], [r_dft[cs]], ch=f"dft{cs}")
                            for g in range(4):
                                pb, r_pb = bank()
                                mm([(pb[:], [(pftm[:, ti, g * 128:(g + 1) * 128], dft[cs][:, ti, :]) for ti in range(NT)])],
                                   [r_pftm, r_dft[cs]], [r_pb])
                                cp("act" if g % 2 else "dve", AB[:, cs, g, :], pb[:], [r_pb], [r_AB[cs]])
                        for g in range(4):
                            pb, r_pb = bank()
                            mm([(pb[:], [(k["cch"][:], AB[:, 0, g, :]), (k["schn"][:], AB[:, 1, g, :])])],
                               [r_AB[0], r_AB[1], kr["cch"], kr["schn"]], [r_pb])
                            cp("act" if g % 2 else "dve", yfT[:, g, tq * 512:(tq + 1) * 512], pb[:], [r_pb], [r_yfT])

                if b == 0:
                    dbgout("yfT", yfT[:], [128, 4, L], BF16, [r_yfT])
                p.fence()
                with ExitStack() as s4:
                    wfo_t = sb(s4, "wfo_t", [128, 4, D], BF16)
                    who_t = sb(s4, "who_t", [128, 4, D], BF16)
                    wo_t = sb(s4, "wo_t", [128, 8, D], BF16)
                    r_wfo_t, r_who_t, r_wo_t = R(), R(), R()
                    dma("sp", wfo_t[:], wfob.rearrange("(kc p) n -> p kc n", p=128), [r_wfob], [r_wfo_t], ch="wm0")
                    dma("sp", who_t[:], whob.rearrange("(kc p) n -> p kc n", p=128), [r_whob], [r_who_t], ch="wm1")
                    dma("sp", wo_t[:], wob.rearrange("(kc p) n -> p kc n", p=128), [r_wob], [r_wo_t], ch="wm2")
                    bc = sb(s4, "bc", [128, 5, D])
                    r_bc = R()
                    for i, j0 in enumerate((16, 32, 24)):
                        dma("sp", bc[:, i, :], modrows[b:b + 1, j0 * 128:(j0 + 8) * 128].partition_broadcast(128), [r_modrows], [r_bc], ch="bc")
                    dma("sp", bc[:, 3, :], ln1_g.partition_broadcast(128), [], [r_bc], ch="bc")
                    dma("sp", bc[:, 4, :], ln1_b.partition_broadcast(128), [], [r_bc], ch="bc")
                    mTs = [sb(s4, f"mT{i}", [128, 8, 512], BF16) for i in range(2)]
                    r_mTs = [R(), R()]
                    sgf = sb(s4, "sgf", [128, 512])
                    sgh = sb(s4, "sgh", [128, 512])
                    r_sgf, r_sgh = R(), R()
                    xr = [sb(s4, f"xr{i}", [128, D]) for i in range(2)]
                    r_xr = [R(), R()]
                    tbuf = sb(s4, "tbuf", [128, D])
                    r_tbuf = R()
                    x1t = sb(s4, "x1t", [128, D])
                    r_x1t = R()
                    u2t = sb(s4, "u2t", [128, D])
                    r_u2t = R()
                    u2bt = sb(s4, "u2bt", [128, D], BF16)
                    r_u2bt = R()
                    u2T = sb(s4, "u2T", [128, 8, 128])
                    r_u2T = R()
                    stats = sb(s4, "stats", [128, 2, 6])
                    mv = sb(s4, "mv", [128, 2])
                    nmr = sb(s4, "nmr", [128, 2])
                    r_stats, r_mv, r_nmr = R(), R(), R()

                    def layer_norm(src, r_src, dst, r_dst, gidx, bidx, bct, r_bct):
                        for hf in range(2):
                            p.op("dve", lambda e, hf=hf, stats=stats, src=src: e.bn_stats(stats[:, hf, :], src[:, hf * 512:(hf + 1) * 512]), reads=[r_src], writes=[r_stats])
                        p.op("dve", lambda e, mv=mv, stats=stats: e.bn_aggr(mv[:], stats[:].rearrange("p a s -> p (a s)")), reads=[r_stats], writes=[r_mv])
                        act(nmr[:, 0:1], mv[:, 1:2], AF.Sqrt, [r_mv, r_eps], [r_nmr], bias=epst[:, 1:2], scale=1.0)
                        p.op("dve", lambda e, nmr=nmr: e.reciprocal(nmr[:, 0:1], nmr[:, 0:1]), reads=[r_nmr], writes=[r_nmr])
                        stt(dst[:], src[:], mv[:, 0:1], bct[:, gidx, :], ALU.subtract, ALU.mult, [r_src, r_mv, r_bct], [r_dst])
                        stt(dst[:], dst[:], nmr[:, 0:1], bct[:, bidx, :], ALU.mult, ALU.add, [r_dst, r_nmr, r_bct], [r_dst])

                    def gates_j(tq, j):
                        wgj = [None, None]
                        r_wgj = [None, None]
                        for br in range(2):
                            wgj[br], r_wgj[br] = load_wg(3072 + br * 1024 + j * 128, 128)
                        for br, (sgt, r_sgt, yT, r_yT, wpt, r_wpt) in enumerate(((sgf, r_sgf, yfT, r_yfT, wfo_t, r_wfo_t),
                                                                               (sgh, r_sgh, yhT, r_yhT, who_t, r_who_t))):
                            pb, r_pb = bank()
                            mm([(pb[:], [(wgj[br][:, kc, 0:128], uT[:, kc, tq * 512:(tq + 1) * 512]) for kc in range(8)])],
                               [r_wgj[br], r_uT], [r_pb])
                            act(sgt[:], pb[:], AF.Sigmoid, [r_pb], [r_sgt])
                            pb2, r_pb2 = bank()
                            mm([(pb2[:], [(wpt[:, kc, j * 128:(j + 1) * 128], yT[:, kc, tq * 512:(tq + 1) * 512]) for kc in range(4)])],
                               [r_wpt, r_yT], [r_pb2])
                            tt("dve", sgt[:], sgt[:], pb2[:], ALU.mult, [r_sgt, r_pb2], [r_sgt])
                        tt("dve", mTs[tq % 2][:, j, :], sgf[:], sgh[:], ALU.add, [r_sgf, r_sgh], [r_mTs[tq % 2]])
                    def ln_tile(tq, a):
                        ti = tq * 4 + a
                        gt = b * NT + ti
                        i = ti % 2
                        dma("sp", xr[i][:], x[b, ti * 128:(ti + 1) * 128, :], [], [r_xr[i]], ch=f"xr{i}")
                        for hf in range(2):
                            pb, r_pb = bank()
                            mm([(pb[:], [(mTs[tq % 2][:, kc, a * 128:(a + 1) * 128], wo_t[:, kc, hf * 512:(hf + 1) * 512]) for kc in range(8)])],
                               [r_mTs[tq % 2], r_wo_t], [r_pb])
                            tt("dve", tbuf[:, hf * 512:(hf + 1) * 512], pb[:], bc[:, 0, hf * 512:(hf + 1) * 512], ALU.mult, [r_pb, r_bc], [r_tbuf])
                        stt(tbuf[:], xr[i][:], ALPHA, tbuf[:], ALU.mult, ALU.add, [r_xr[i], r_tbuf], [r_tbuf])
                        layer_norm(tbuf, r_tbuf, x1t, r_x1t, 3, 4, bc, r_bc)
                        dma("sp", x1s[gt * 128:(gt + 1) * 128, :], x1t[:], [r_x1t], [], ch="x1s", wacc=[r_x1s])
                        if debug:
                            dma("pool", dbg["x1"][gt * 128:(gt + 1) * 128, :], x1t[:], [r_x1t], [R()], ch="dbg")
                        tt("dve", u2t[:], x1t[:], bc[:, 1, :], ALU.mult, [r_x1t, r_bc], [r_u2t])
                        tt("dve", u2t[:], u2t[:], bc[:, 2, :], ALU.add, [r_u2t, r_bc], [r_u2t])
                        cp("act", u2bt[:], u2t[:], [r_u2t], [r_u2bt])
                        dma("sp", u2b[gt * 128:(gt + 1) * 128, :], u2bt[:], [r_u2bt], [], ch="u2b", wacc=[r_u2b])
                        for hf in range(2):
                            pb, r_pb = bank()
                            for kk in range(4):
                                kc = hf * 4 + kk
                                tr(pb[:, kk * 128:(kk + 1) * 128], u2t[:, kc * 128:(kc + 1) * 128], k["ident_f"][:], [r_u2t, kr["ident_f"]], [r_pb])
                            cp("act", u2T[:, hf * 4:(hf + 1) * 4, :].rearrange("p a t -> p (a t)"), pb[:], [r_pb], [r_u2T])
                        pb, r_pb = bank()
                        mm([(pb[:, 0:NE], [(u2T[:, kc, :], wr[:, kc, :]) for kc in range(8)])], [r_u2T, r_wr], [r_pb])
                        tt("dve", Ltab[:, gt, :], pb[:, 0:NE], brt[:], ALU.add, [r_pb, r_brt], [r_tab[gt]])
                        p.op("dve", lambda e, gt=gt: e.max(Vtab[:, gt, :], Ltab[:, gt, :]), reads=[r_tab[gt]], writes=[r_tab[gt]])

                    for j in range(8):
                        gates_j(0, j)
                    for tq in range(4):
                        for a in range(4):
                            if tq + 1 < 4:
                                gates_j(tq + 1, 2 * a)
                                gates_j(tq + 1, 2 * a + 1)
                            ln_tile(tq, a)
        issue_casts(len(pending_casts))
        p.fence()
        dbgout("Ltab", Ltab[:], [128, NTT, NE], F32, r_tab)
        dbgout("Vtab", Vtab[:], [128, NTT, 8], F32, r_tab)
        desti = sb(es, "desti", [128, 4, NTT], I32)
        r_desti = R()
        Wtab = sb(es, "Wtab", [128, NTT, 4])
        r_Wtab = R()
        idxw = sb(es, "idxw", [128, NBLK], I32)
        idxe = sb(es, "idxe", [128, NBLK], I32)
        r_idx = R()
        ALLTAB = r_tab

        with ExitStack() as sd:
            pos = sb(sd, "pos", [128, NTT, NE])
            r_pos = R()
            Mtab = sb(sd, "Mtab", [128, NTT, NE])
            r_M = R()
            tt("dve", Mtab[:], Ltab[:], Vtab[:, :, 3:4].to_broadcast([128, NTT, NE]), ALU.is_ge, r_tab, [r_M])
            k["le_mask"] = sb(sd, "c_le_mask", [128, NE * NE])
            kr["le_mask"] = R()
            dma("sp", k["le_mask"][:], cd["le_mask"], [], [kr["le_mask"]], ch="const")
            run = sb(sd, "run", [128, NE])
            r_run = R()
            p.op("dve", lambda e: e.memset(run[:], 0.0), writes=[r_run])
            for gt in range(NTT):
                pb, r_pb = bank()
                mm([(pb[:, 0:NE], [(k["ltri"][:], Mtab[:, gt, :])]), (pb[:, NE:2 * NE], [(k["ones_f"][:], Mtab[:, gt, :])])],
                   [r_M, kr["ltri"], kr["ones_f"]], [r_pb])
                tt("dve", pos[:, gt, :], pb[:, 0:NE], run[:], ALU.add, [r_pb, r_run], [r_pos])
                tt("dve", run[:], pb[:, NE:2 * NE], run[:], ALU.add, [r_pb, r_run], [r_run])
            big = sb(sd, "big", [128, NE * NBLK])
            r_big = R()
            nblk = sb(sd, "nblk", [128, NE])
            padded = sb(sd, "padded", [128, NE])
            pend = sb(sd, "pend", [128, NE])
            pstart = sb(sd, "pstart", [128, NE])
            r_sm = R()
            tt("dve", big[:].rearrange("p (e j) -> p e j", e=NE), run[:].unsqueeze(2).to_broadcast([128, NE, NBLK]),
               k["blk_thr"][:].unsqueeze(1).to_broadcast([128, NE, NBLK]), ALU.is_gt, [r_run, kr["blk_thr"]], [r_big])
            p.op("dve", lambda e: e.reduce_sum(nblk[:], big[:].rearrange("p (e j) -> p e j", e=NE), axis=AX.X), reads=[r_big], writes=[r_sm])
            ts("dve", padded[:], nblk[:], float(BLK), None, ALU.mult, None, [r_sm], [r_sm])
            tt("dve", big[:, 0:NE * NE].rearrange("p (e f) -> p e f", e=NE), padded[:].unsqueeze(1).to_broadcast([128, NE, NE]),
               k["le_mask"][:].rearrange("p (e f) -> p e f", e=NE), ALU.mult, [r_sm, kr["le_mask"], r_big], [r_big])
            p.op("dve", lambda e: e.reduce_sum(pend[:], big[:, 0:NE * NE].rearrange("p (e f) -> p e f", e=NE), axis=AX.X), reads=[r_big], writes=[r_sm])
            tt("dve", pstart[:], pend[:], padded[:], ALU.subtract, [r_sm], [r_sm])
            tt("dve", big[:].rearrange("p (j e) -> p j e", e=NE), pend[:].unsqueeze(1).to_broadcast([128, NBLK, NE]),
               k["blk_thr"][:].unsqueeze(2).to_broadcast([128, NBLK, NE]), ALU.is_le, [r_sm, kr["blk_thr"], r_big], [r_big])
            bef = sb(sd, "bef", [128, NBLK])
            r_bef = R()
            p.op("dve", lambda e: e.reduce_sum(bef[:], big[:].rearrange("p (j e) -> p j e", e=NE), axis=AX.X), reads=[r_big], writes=[r_bef])
            ts("dve", bef[:], bef[:], float(NE - 1), None, ALU.min, None, [r_bef], [r_bef])
            cp("dve", idxe[:], bef[:], [r_bef], [r_idx])
            ts("dve", bef[:], bef[:], 128.0, k["iota_p"][:, 0:1], ALU.mult, ALU.add, [r_bef, kr["iota_p"]], [r_bef])
            cp("dve", idxw[:], bef[:], [r_bef], [r_idx])
            tt("dve", pos[:], pos[:], pstart[:].unsqueeze(1).to_broadcast([128, NTT, NE]), ALU.add, [r_pos, r_sm], [r_pos])
            oh = sb(sd, "oh", [128, NTT, NE])
            r_oh = R()
            destf = sb(sd, "destf", [128, 4, NTT])
            r_destf = R()
            for kk in range(4):
                tt("dve", oh[:], Ltab[:], Vtab[:, :, kk:kk + 1].to_broadcast([128, NTT, NE]), ALU.is_equal, ALLTAB, [r_oh])
                tt("dve", oh[:], oh[:], pos[:], ALU.mult, [r_oh, r_pos], [r_oh])
                p.op("dve", lambda e, kk=kk: e.reduce_sum(destf[:, kk, :], oh[:], axis=AX.X), reads=[r_oh], writes=[r_destf])
            cp("dve", desti[:], destf[:], [r_destf], [r_desti])
            ew = sb(sd, "ew", [128, NTT, 4])
            r_ew = R()
            ssum = sb(sd, "ssum", [128, NTT])
            r_ssum = R()
            tt("dve", ew[:], Vtab[:, :, 0:4], Vtab[:, :, 0:1].to_broadcast([128, NTT, 4]), ALU.subtract, ALLTAB, [r_ew])
            act(ew[:], ew[:], AF.Exp, [r_ew], [r_ew])
            p.op("dve", lambda e: e.reduce_sum(ssum[:], ew[:], axis=AX.X), reads=[r_ew], writes=[r_ssum])
            p.op("dve", lambda e: e.reciprocal(ssum[:], ssum[:]), reads=[r_ssum], writes=[r_ssum])
            tt("dve", Wtab[:], ew[:], ssum[:].unsqueeze(2).to_broadcast([128, NTT, 4]), ALU.mult, [r_ew, r_ssum], [r_Wtab])
            ub = [sb(sd, f"ub{i}", [128, D], BF16) for i in range(2)]
            r_ub = [R(), R()]
            for gt in range(NTT):
                i = gt % 2
                dma("sp", ub[i][:], u2b[gt * 128:(gt + 1) * 128, :], [r_u2b], [r_ub[i]], ch=f"ub{i}")
                for kk in range(4):
                    p.op("pool", lambda e, i=i, kk=kk, gt=gt: e.indirect_dma_start(
                        out=xs, out_offset=bass.IndirectOffsetOnAxis(ap=desti[:, kk, gt:gt + 1], axis=0), in_=ub[i][:], in_offset=None),
                        reads=[r_ub[i], r_desti, r_zf], wacc=[r_xs], ch=f"sc{i}")

        dbgout("desti", desti[:], [128, 4, NTT], I32, [r_desti])
        dbgout("Wtab", Wtab[:], [128, NTT, 4], F32, [r_Wtab])
        dbgout("idxw", idxw[:], [128, NBLK], I32, [r_idx])
        p.fence()
        with ExitStack() as se:
            wgu_t = [sb(se, f"wgu_t{i}", [128, 8 * 2 * D], BF16) for i in range(2)]
            wdn_t = [sb(se, f"wdn_t{i}", [128, 8 * D], BF16) for i in range(2)]
            bgu_t = [sb(se, f"bgu_t{i}", [128, 16]) for i in range(2)]
            bdn_t = [sb(se, f"bdn_t{i}", [128, D]) for i in range(2)]
            r_wt = [R(), R()]
            xrows = [sb(se, f"xrows{i}", [128, 4, D], BF16) for i in range(2)]
            r_xrows = [R(), R()]
            xsT = [sb(se, f"xsT{i}", [128, 8, BLK], BF16) for i in range(2)]
            r_xsT = [R(), R()]
            actT = sb(se, "actT", [128, 8, BLK], BF16)
            r_actT = [R() for _ in range(8)]
            gc = [sb(se, f"gc{i}", [128, BLK]) for i in range(2)]
            sgm = [sb(se, f"sgm{i}", [128, BLK]) for i in range(2)]
            uu = [sb(se, f"uu{i}", [128, BLK]) for i in range(2)]
            r_gc, r_sgm, r_uu = [R(), R()], [R(), R()], [R(), R()]
            yst = [sb(se, f"yst{i}", [128, D]) for i in range(2)]
            r_yst = [R(), R()]

            def load_block(j):
                i = j % 2
                def g(dst, src, idx):
                    p.op("pool", lambda e: e.indirect_dma_start(out=dst, out_offset=None, in_=src,
                                                               in_offset=bass.IndirectOffsetOnAxis(ap=idx, axis=0)),
                         reads=[r_idx, r_wgub, r_wdnb], wacc=[r_wt[i]], ch=f"wt{i}")
                g(wgu_t[i][:], wgub, idxw[:, j:j + 1])
                g(wdn_t[i][:], wdnb, idxw[:, j:j + 1])
                g(bgu_t[i][:], bguT, idxw[:, j:j + 1])
                g(bdn_t[i][:], b_dn, idxe[:, j:j + 1])
                dma("sp", xrows[i][:], xs[j * BLK:(j + 1) * BLK, :].rearrange("(a p) d -> p a d", p=128), [r_xs], [r_xrows[i]], ch=f"xrows{i}")

            def transposes(j):
                i = j % 2
                for kc in range(8):
                    pb, r_pb = bank()
                    mm([(pb[:, a * 128:(a + 1) * 128], [(xrows[i][:, a, kc * 128:(kc + 1) * 128], k["ident_b"][:])]) for a in range(4)],
                       [r_xrows[i], kr["ident_b"]], [r_pb])
                    cp("act" if kc % 2 else "dve", xsT[i][:, kc, :], pb[:], [r_pb], [r_xsT[i]])

            load_block(0)
            transposes(0)
            for j in range(NBLK):
                i = j % 2
                if j + 1 < NBLK:
                    load_block(j + 1)
                for fj in range(8):
                    q_ = fj % 2
                    pg_, r_pg = bank()
                    mm([(pg_[:], [(wgu_t[i][:, kc * 2048 + fj * 128:kc * 2048 + (fj + 1) * 128], xsT[i][:, kc, :]) for kc in range(8)])],
                       [r_wt[i], r_xsT[i]], [r_pg])
                    pu_, r_pu = bank()
                    mm([(pu_[:], [(wgu_t[i][:, kc * 2048 + 1024 + fj * 128:kc * 2048 + 1024 + (fj + 1) * 128], xsT[i][:, kc, :]) for kc in range(8)])],
                       [r_wt[i], r_xsT[i]], [r_pu])
                    ts("dve", gc[q_][:], pg_[:], bgu_t[i][:, fj:fj + 1], 7.0, ALU.add, ALU.min, [r_pg, r_wt[i]], [r_gc[q_]])
                    act(sgm[q_][:], gc[q_][:], AF.Sigmoid, [r_gc[q_]], [r_sgm[q_]], scale=1.702)
                    act(uu[q_][:], pu_[:], AF.Identity, [r_pu, r_wt[i]], [r_uu[q_]], bias=bgu_t[i][:, 8 + fj:9 + fj], scale=1.0)
                    ts("dve", uu[q_][:], uu[q_][:], 7.0, -7.0, ALU.min, ALU.max, [r_uu[q_]], [r_uu[q_]])
                    tt("pool", gc[q_][:], gc[q_][:], sgm[q_][:], ALU.mult, [r_gc[q_], r_sgm[q_]], [r_gc[q_]])
                    stt(actT[:, fj, :], uu[q_][:], 1.0, gc[q_][:], ALU.add, ALU.mult, [r_gc[q_], r_uu[q_]], [r_actT[fj]])
                if j + 1 < NBLK:
                    transposes(j + 1)
                for a in range(4):
                    q_ = a % 2
                    for hf in range(2):
                        pb, r_pb = bank()
                        mm([(pb[:], [(actT[:, fc, a * 128:(a + 1) * 128], wdn_t[i][:, fc * 1024 + hf * 512:fc * 1024 + (hf + 1) * 512]) for fc in range(8)])],
                           r_actT + [r_wt[i]], [r_pb])
                        tt("dve", yst[q_][:, hf * 512:(hf + 1) * 512], pb[:], bdn_t[i][:, hf * 512:(hf + 1) * 512], ALU.add, [r_pb, r_wt[i]], [r_yst[q_]])
                    dma("sp", ys[j * BLK + a * 128:j * BLK + (a + 1) * 128, :], yst[q_][:], [r_yst[q_]], [], ch=f"yst{q_}", wacc=[r_ys])

        p.fence()
        with ExitStack() as sc_:
            bc2 = sb(sc_, "bc2", [128, NB + 2, D])
            r_bc2 = R()
            for b in range(NB):
                dma("sp", bc2[:, b, :], modrows[b:b + 1, 40 * 128:48 * 128].partition_broadcast(128), [r_modrows], [r_bc2], ch="bc2")
            dma("sp", bc2[:, NB, :], ln2_g.partition_broadcast(128), [], [r_bc2], ch="bc2")
            dma("sp", bc2[:, NB + 1, :], ln2_b.partition_broadcast(128), [], [r_bc2], ch="bc2")
            yg = [sb(sc_, f"yg{i}", [128, 4, D]) for i in range(2)]
            r_yg = [R(), R()]
            x1r = [sb(sc_, f"x1r{i}", [128, D]) for i in range(2)]
            r_x1r = [R(), R()]
            wd = [sb(sc_, f"wd{i}", [128, 4, 128]) for i in range(2)]
            r_wd = [R(), R()]
            ffs = [sb(sc_, f"ff{i}", [128, D]) for i in range(2)]
            r_ffs = [R(), R()]
            ot = [sb(sc_, f"ot{i}", [128, D]) for i in range(2)]
            r_ot = [R(), R()]
            stats2s = [sb(sc_, f"stats2{i}", [128, 2, 6]) for i in range(2)]
            mv2s = [sb(sc_, f"mv2{i}", [128, 2]) for i in range(2)]
            nmr2s = [sb(sc_, f"nmr2{i}", [128, 2]) for i in range(2)]
            r_stats2s, r_mv2s, r_nmr2s = [R(), R()], [R(), R()], [R(), R()]
            r_out = R()
            outf = out.rearrange("b l d -> (b l) d")

            def load_c(gt):
                i = gt % 2
                for kk in range(4):
                    p.op("pool", lambda e, kk=kk: e.indirect_dma_start(out=yg[i][:, kk, :], out_offset=None, in_=ys,
                                                                      in_offset=bass.IndirectOffsetOnAxis(ap=desti[:, kk, gt:gt + 1], axis=0)),
                         reads=[r_desti, r_ys], wacc=[r_yg[i]], ch=f"yg{i}")
                dma("sp", x1r[i][:], x1s[gt * 128:(gt + 1) * 128, :], [r_x1s], [r_x1r[i]], ch=f"x1r{i}")

            load_c(0)
            for gt in range(NTT):
                i = gt % 2
                b = gt // NT
                if gt + 1 < NTT:
                    load_c(gt + 1)
                ff, r_ff = ffs[i], r_ffs[i]
                stats2, mv2, nmr2 = stats2s[i], mv2s[i], nmr2s[i]
                r_stats2, r_mv2, r_nmr2 = r_stats2s[i], r_mv2s[i], r_nmr2s[i]
                tt("dve", wd[i][:], k["ident_f"][:].unsqueeze(1).to_broadcast([128, 4, 128]),
                   Wtab[:, gt, :].unsqueeze(2).to_broadcast([128, 4, 128]), ALU.mult, [kr["ident_f"], r_Wtab], [r_wd[i]])
                for hf in range(2):
                    pb, r_pb = bank()
                    mm([(pb[:], [(wd[i][:, kk, :], yg[i][:, kk, hf * 512:(hf + 1) * 512]) for kk in range(4)])], [r_wd[i], r_yg[i]], [r_pb])
                    tt("dve", ff[:, hf * 512:(hf + 1) * 512], pb[:], bc2[:, b, hf * 512:(hf + 1) * 512], ALU.mult, [r_pb, r_bc2], [r_ff])
                stt(ff[:], x1r[i][:], ALPHA, ff[:], ALU.mult, ALU.add, [r_x1r[i], r_ff], [r_ff])
                for hf in range(2):
                    p.op("dve", lambda e, hf=hf, stats2=stats2, ff=ff: e.bn_stats(stats2[:, hf, :], ff[:, hf * 512:(hf + 1) * 512]), reads=[r_ff], writes=[r_stats2])
                p.op("dve", lambda e, mv2=mv2, stats2=stats2: e.bn_aggr(mv2[:], stats2[:].rearrange("p a s -> p (a s)")), reads=[r_stats2], writes=[r_mv2])
                act(nmr2[:, 0:1], mv2[:, 1:2], AF.Sqrt, [r_mv2, r_eps], [r_nmr2], bias=epst[:, 1:2], scale=1.0)
                p.op("dve", lambda e, nmr2=nmr2: e.reciprocal(nmr2[:, 0:1], nmr2[:, 0:1]), reads=[r_nmr2], writes=[r_nmr2])
                stt(nmr2[:, 1:2], mv2[:, 0:1], -1.0, nmr2[:, 0:1], ALU.mult, ALU.mult, [r_mv2, r_nmr2], [r_nmr2])
                act(ot[i][:], ff[:], AF.Identity, [r_ff, r_nmr2], [r_ot[i]], bias=nmr2[:, 1:2], scale=nmr2[:, 0:1])
                tt("pool", ot[i][:], ot[i][:], bc2[:, NB, :], ALU.mult, [r_ot[i], r_bc2], [r_ot[i]])
                tt("dve", ot[i][:], ot[i][:], bc2[:, NB + 1, :], ALU.add, [r_ot[i], r_bc2], [r_ot[i]])
                dma("sp", outf[gt * 128:(gt + 1) * 128, :], ot[i][:], [r_ot[i]], [], ch=f"ot{i}", wacc=[r_out])
            p.final_wait("sp", [r_out])
        p.emit()
    return nc


_NC = {}


def kernel(**inputs):
    debug = bool(inputs.pop("_debug", False))
    if debug not in _NC:
        _NC[debug] = build(debug)
    nc = _NC[debug]
    f = lambda a: np.ascontiguousarray(np.asarray(a, dtype=np.float32))
    consts = _consts()
    shared = {
        "w_ada": f(inputs["w_ada"][0]),
        "b_adaT": f(np.asarray(inputs["b_ada"][0]).reshape(48, 128).T),
        "b_ada": f(inputs["b_ada"]),
        "w_in": f(inputs["w_in"][0]),
        "lb_raw": f(inputs["lb_raw"]),
        "normgT": f(np.asarray(inputs["hg_norm_g"][0]).reshape(4, 128).T),
        "w_four_out": f(inputs["w_four_out"][0]),
        "w_hg_out": f(inputs["w_hg_out"][0]),
        "w_o": f(inputs["w_o"][0]),
        "ln1_g": f(inputs["ln1_g"]), "ln1_b": f(inputs["ln1_b"]),
        "w_router": f(inputs["w_router"][0]), "b_router": f(inputs["b_router"]),
        "w_gate_up": f(inputs["w_gate_up"][0]), "b_guT": f(np.asarray(inputs["b_gate_up"][0]).reshape(NE, 16, 128).transpose(0, 2, 1).reshape(NE * 128, 16)),
        "w_down": f(inputs["w_down"][0]), "b_down": f(inputs["b_down"][0]),
        "ln2_g": f(inputs["ln2_g"]), "ln2_b": f(inputs["ln2_b"]),
    }
    for kname, v in consts.items():
        shared["k_" + kname] = v
    x = np.asarray(inputs["x"], dtype=np.float32)
    ctx = np.asarray(inputs["ctx"], dtype=np.float32)
    c = np.asarray(inputs["c"], dtype=np.float32)
    c_ctx = np.asarray(inputs["c_ctx"], dtype=np.float32)
    in_maps = []
    for core in range(NCORES):
        sl = slice(core * NB, (core + 1) * NB)
        c5 = np.concatenate([c[sl], c_ctx[None, :]], axis=0)
        c5 = np.ascontiguousarray(c5.reshape(5, 8, 128).transpose(2, 1, 0))
        m = dict(shared)
        m["x"] = np.ascontiguousarray(x[sl])
        m["ctx"] = np.ascontiguousarray(ctx[sl])
        m["c5"] = c5
        in_maps.append(m)
    res = run_bass_kernel_spmd(nc, in_maps, core_ids=list(range(NCORES)))
    outs = [np.asarray(r["out"], dtype=np.float32) for r in res.results]
    full = np.concatenate(outs, axis=0)
    if debug:
        return full, res.results
    return full
```

```python
import numpy as np
import ml_dtypes
import concourse.bass as bass
import concourse.mybir as mybir
from contextlib import ExitStack
from concourse.bass_utils import run_bass_kernel_spmd

F32 = mybir.dt.float32
BF16 = mybir.dt.bfloat16
I32 = mybir.dt.int32
AF = mybir.ActivationFunctionType
ALU = mybir.AluOpType
AX = mybir.AxisListType

NCORES = 8
NB = 4
L = 2048
LC = 256
D = 1024
NT = L // 128
NTOK = NB * L
NTT = NTOK // 128
NE = 32
BLK = 512
NBLK = NTOK * 4 // BLK + NE
NROWS = NBLK * BLK
ALPHA = 2.0 ** 0.25
LN_EPS = 1e-5
RMS_EPS = 1e-6


class R:
    __slots__ = ("name", "w", "r")

    def __init__(self, name=""):
        self.name = name
        self.w = {}
        self.r = {}


class Prog:
    ENG = ("pe", "act", "dve", "pool", "sp")

    def __init__(self, nc, es):
        self.nc = nc
        self.es = es
        self.q = {e: [] for e in self.ENG}
        self.semh = {}
        self.cnt = {}
        self.seen = {e: {} for e in self.ENG}
        for e in self.ENG:
            self._sem("c_" + e)

    def _sem(self, name):
        if name not in self.semh:
            self.semh[name] = self.es.enter_context(self.nc.semaphore(name))
            self.cnt[name] = 0
        return self.semh[name]

    def op(self, eng, fn, reads=(), writes=(), ch=None, wacc=()):
        waits = {}

        def merge(d):
            for s, v in d.items():
                if waits.get(s, 0) < v:
                    waits[s] = v
        for r in reads:
            merge(r.w)
        for r in writes:
            merge(r.w)
            merge(r.r)
        for r in wacc:
            merge(r.r)
        own = "c_" + eng
        seen = self.seen[eng]
        wl = []
        for s, v in waits.items():
            if s == own and eng == "pe":
                continue
            if seen.get(s, 0) >= v:
                continue
            seen[s] = v
            wl.append((s, v))
        if ch is None:
            sname, inc = own, 1
        else:
            sname, inc = "d_" + ch, 16
            self._sem(sname)
        self.cnt[sname] += inc
        val = self.cnt[sname]
        self.q[eng].append((wl, fn, sname, inc))
        for r in reads:
            if r.r.get(sname, 0) < val:
                r.r[sname] = val
        for r in writes:
            r.w = {sname: val}
            r.r = {}
        for r in wacc:
            r.w[sname] = val

    def fence(self):
        snap = dict(self.cnt)
        for eng in self.ENG:
            wl = []
            seen = self.seen[eng]
            for s, v in snap.items():
                if v == 0 or seen.get(s, 0) >= v:
                    continue
                if s == "c_" + eng or s in ("d_cast", "d_dbg", "d_zf"):
                    continue
                seen[s] = v
                wl.append((s, v))
            if wl:
                self.q[eng].append((wl, None, None, 0))

    def final_wait(self, eng, resources):
        waits = {}
        for r in resources:
            for s, v in list(r.w.items()) + list(r.r.items()):
                if waits.get(s, 0) < v:
                    waits[s] = v
        self.q[eng].append((list(waits.items()), None, None, 0))

    def emit(self):
        nc = self.nc
        with nc.Block() as block:
            def replay(name, engobj):
                for wl, fn, sname, inc in self.q[name]:
                    for s, v in wl:
                        engobj.wait_ge(self.semh[s], v)
                    if fn is not None:
                        fn(engobj).then_inc(self.semh[sname], inc)

            @block.tensor
            def _(e):
                replay("pe", e)

            @block.scalar
            def _(e):
                replay("act", e)

            @block.vector
            def _(e):
                replay("dve", e)

            @block.gpsimd
            def _(e):
                replay("pool", e)

            @block.sync
            def _(e):
                replay("sp", e)


def _consts():
    c = {}
    c["ident_f"] = np.eye(128, dtype=np.float32)
    c["ident_b"] = np.eye(128, dtype=np.float32).astype(ml_dtypes.bfloat16)
    t = np.arange(128)
    same = (t[:, None] // 64) == (t[None, :] // 64)
    inc_f = (same & (t[:, None] <= t[None, :])).astype(np.float32)
    su_f = (same & (t[:, None] > t[None, :])).astype(np.float32)
    c["tri"] = np.stack([np.stack([inc_f, su_f, -inc_f]), np.stack([inc_f.T, su_f.T, -inc_f.T])]).astype(np.float32)
    c["tri"] = np.ascontiguousarray(c["tri"].transpose(2, 0, 1, 3)).astype(ml_dtypes.bfloat16)
    c["ones_f"] = np.ones((128, 128), np.float32)
    tt = np.arange(L)
    r, w = tt // 64, tt % 64
    ph = (np.outer(r, r) / 32.0 + np.outer(w, w) / 64.0) * 2 * np.pi
    c["ctok"] = (np.cos(ph) / np.sqrt(L)).astype(ml_dtypes.bfloat16)
    c["stok"] = (np.sin(ph) / np.sqrt(L)).astype(ml_dtypes.bfloat16)
    cc = np.arange(128)
    phc = np.outer(cc, cc) * 2 * np.pi / 128.0
    c["cch"] = (np.cos(phc) / np.sqrt(128)).astype(ml_dtypes.bfloat16)
    c["schn"] = (-np.sin(phc) / np.sqrt(128)).astype(ml_dtypes.bfloat16)
    c["ltri"] = (t[:, None] < t[None, :]).astype(np.float32)
    ee = np.arange(NE)
    c["le_mask"] = np.tile((ee[None, :] <= ee[:, None]).astype(np.float32).reshape(1, NE * NE), (128, 1))
    c["ones_b"] = np.ones((128, 128), np.float32).astype(ml_dtypes.bfloat16)
    c["iota_e"] = np.tile(np.arange(NE, dtype=np.float32)[None, :], (128, 1))
    c["iota_p"] = np.arange(128, dtype=np.float32)[:, None].copy()
    c["blk_thr"] = np.tile((np.arange(NBLK, dtype=np.float32) * BLK)[None, :], (128, 1))
    return c


CONST_DT = {"ident_f": F32, "ident_b": BF16, "tri": BF16, "ones_f": F32, "ctok": BF16, "stok": BF16,
            "cch": BF16, "schn": BF16, "ltri": F32, "le_mask": F32, "ones_b": BF16, "iota_e": F32, "iota_p": F32, "blk_thr": F32}


def build(debug=False):
    nc = bass.Bass("TRN2", target_bir_lowering=False)
    consts = _consts()

    def din(name, shape, dt=F32):
        return nc.dram_tensor(name, list(shape), dt, kind="ExternalInput").ap()

    def dscr(name, shape, dt):
        return nc.dram_tensor(name, list(shape), dt, kind="Internal").ap()

    x = din("x", [NB, L, D])
    ctx = din("ctx", [NB, LC, D])
    c5 = din("c5", [128, 8, 5])
    w_ada = din("w_ada", [D, 6 * D])
    b_adaT = din("b_adaT", [128, 48])
    b_ada = din("b_ada", [1, 6 * D])
    w_in = din("w_in", [D, 5120])
    lb_raw = din("lb_raw", [2, 2, 512])
    normgT = din("normgT", [128, 4])
    w_fo = din("w_four_out", [512, D])
    w_ho = din("w_hg_out", [512, D])
    w_o = din("w_o", [D, D])
    ln1_g = din("ln1_g", [1, D])
    ln1_b = din("ln1_b", [1, D])
    w_router = din("w_router", [D, NE])
    b_router = din("b_router", [1, NE])
    w_gu = din("w_gate_up", [NE, D, 2 * D])
    bguT = din("b_guT", [NE * 128, 16])
    w_dn = din("w_down", [NE, D, D])
    b_dn = din("b_down", [NE, D])
    ln2_g = din("ln2_g", [1, D])
    ln2_b = din("ln2_b", [1, D])
    cd = {k: din("k_" + k, v.shape, CONST_DT[k]) for k, v in consts.items()}
    out = nc.dram_tensor("out", [NB, L, D], F32, kind="ExternalOutput").ap()

    winb = dscr("winb", [D, 5120], BF16)
    wfob = dscr("wfob", [512, D], BF16)
    whob = dscr("whob", [512, D], BF16)
    wob = dscr("wob", [D, D], BF16)
    wgub = dscr("wgub", [NE * 128, 8 * 2 * D], BF16)
    wdnb = dscr("wdnb", [NE * 128, 8 * D], BF16)
    modrows = dscr("modrows", [5, 6 * D], F32)
    x1s = dscr("x1s", [NTOK, D], F32)
    u2b = dscr("u2b", [NTOK, D], BF16)
    xs = dscr("xs", [NROWS, D], BF16)
    ys = dscr("ys", [NROWS, D], F32)
    dbg = {}
    if debug:
        dbg["x1"] = nc.dram_tensor("dbg_x1", [NTOK, D], F32, kind="ExternalOutput").ap()
        dbg["yh"] = nc.dram_tensor("dbg_yh", [128, 4, L], F32, kind="ExternalOutput").ap()
        dbg["yf"] = nc.dram_tensor("dbg_yf", [128, 4, L], F32, kind="ExternalOutput").ap()
        dbg["mod"] = nc.dram_tensor("dbg_mod", [128, 48, 5], F32, kind="ExternalOutput").ap()

    with ExitStack() as es:
        p = Prog(nc, es)

        uniq = [0]

        def sb(scope, name, shape, dt=F32):
            uniq[0] += 1
            return scope.enter_context(nc.sbuf_tensor(f"{name}_{uniq[0]}", list(shape), dt))

        ps = [es.enter_context(nc.psum_tensor(f"ps{i}", [128, 512], F32)) for i in range(8)]
        psr = [R(f"ps{i}") for i in range(8)]
        bank_i = [0]

        bank_skip = [None]

        def bank():
            i = bank_i[0]
            if i == bank_skip[0]:
                i = (i + 1) % 8
            bank_i[0] = (i + 1) % 8
            return ps[i], psr[i]

        def mm(groups, reads, writes):
            def fn(e):
                inst = None
                for out_ap, pairs in groups:
                    n = len(pairs)
                    for i, (l, r_) in enumerate(pairs):
                        inst = e.matmul(out_ap, l, r_, start=(i == 0), stop=(i == n - 1))
                return inst
            p.op("pe", fn, reads=reads, writes=writes)

        def tr(out_ap, in_ap, ident, reads, writes):
            p.op("pe", lambda e: e.transpose(out_ap, in_ap, ident), reads=reads, writes=writes)

        def act(out_ap, in_ap, func, reads, writes, bias=None, scale=None):
            kw = {}
            if bias is not None:
                kw["bias"] = bias
            if scale is not None:
                kw["scale"] = scale
            p.op("act", lambda e: e.activation(out_ap, in_ap, func, **kw), reads=reads, writes=writes)

        def tt(eng, out_ap, a, b, op, reads, writes):
            p.op(eng, lambda e: e.tensor_tensor(out_ap, a, b, op), reads=reads, writes=writes)

        def ts(eng, out_ap, a, s1, s2, op0, op1, reads, writes):
            if op1 is None:
                p.op(eng, lambda e: e.tensor_scalar(out_ap, a, s1, None, op0), reads=reads, writes=writes)
            else:
                p.op(eng, lambda e: e.tensor_scalar(out_ap, a, s1, s2, op0, op1), reads=reads, writes=writes)

        def stt(out_ap, a, s, b, op0, op1, reads, writes):
            p.op("dve", lambda e: e.scalar_tensor_tensor(out_ap, a, s, b, op0, op1), reads=reads, writes=writes)

        def cp(eng, out_ap, in_ap, reads, writes):
            if eng == "act":
                p.op("act", lambda e: e.copy(out_ap, in_ap), reads=reads, writes=writes)
            else:
                p.op(eng, lambda e: e.tensor_copy(out_ap, in_ap), reads=reads, writes=writes)

        def dma(eng, out_ap, in_ap, reads, writes, ch, wacc=(), **kw):
            p.op(eng, lambda e: e.dma_start(out=out_ap, in_=in_ap, **kw), reads=reads, writes=writes, ch=ch, wacc=wacc)

        def dbgout(name, ap, shape, dt, reads):
            if not debug:
                return
            t = nc.dram_tensor("dbg_" + name, list(shape), dt, kind="ExternalOutput").ap()
            dma("pool", t, ap, reads, [R()], ch="dbg")

        k = {}
        kr = {}
        for name, arr in consts.items():
            if name in ("ctok", "stok", "le_mask"):
                continue
            k[name] = sb(es, "c_" + name, arr.shape, CONST_DT[name])
            kr[name] = R(name)
            dma("sp", k[name][:], cd[name], [], [kr[name]], ch="const")
        KR = list(kr.values())

        r_winb, r_wfob, r_whob, r_wob, r_wgub, r_wdnb = [R() for _ in range(6)]
        for j in range(10):
            dma("pool", winb[:, j * 512:(j + 1) * 512], w_in[:, j * 512:(j + 1) * 512], [], [], ch="cast_in", wacc=[r_winb])
        dma("pool", wfob, w_fo, [], [r_wfob], ch="cast_fo")
        dma("pool", whob, w_ho, [], [r_whob], ch="cast_ho")
        dma("pool", wob, w_o, [], [r_wob], ch="cast_o")
        zt = sb(es, "zt", [128, D], BF16)
        r_zt = R()
        r_zf = R()
        p.op("dve", lambda e: e.memset(zt[:], 0.0), writes=[r_zt])
        ZR = 4096
        for c_ in range(NROWS // ZR):
            dma("pool", xs[c_ * ZR:(c_ + 1) * ZR, :].rearrange("(n p) d -> p n d", p=128),
                zt[:].unsqueeze(1).to_broadcast([128, ZR // 128, D]), [r_zt], [], ch="zf", wacc=[r_zf])
        modT = sb(es, "modT", [128, 48, 5])
        r_modT = R()
        r_modrows = R()
        lbt = sb(es, "lbt", [128, 2, 512])
        r_lbt = R()
        normg = sb(es, "normg", [128, 4])
        r_normg = R()
        dma("sp", normg[:], normgT, [], [r_normg], ch="const")
        wr = sb(es, "wr", [128, 8, NE])
        r_wr = R()
        dma("sp", wr[:], w_router.rearrange("(kc p) e -> p kc e", p=128), [], [r_wr], ch="const")
        brt = sb(es, "brt", [128, NE])
        r_brt = R()
        dma("sp", brt[:], b_router.partition_broadcast(128), [], [r_brt], ch="const")
        for r_ in KR + [r_normg, r_wr, r_brt]:
            r_.w = {"d_const": p.cnt["d_const"]}
        epst = sb(es, "epst", [128, 3])
        r_eps = R()
        p.op("dve", lambda e: e.memset(epst[:, 0:1], RMS_EPS), writes=[r_eps])
        p.op("dve", lambda e: e.memset(epst[:, 1:2], LN_EPS), writes=[r_eps])
        p.op("dve", lambda e: e.memset(epst[:, 2:3], 1.0), writes=[r_eps])
        r_x1s, r_u2b, r_xs, r_ys = R(), R(), R(), R()
        Ltab = sb(es, "Ltab", [128, NTT, NE])
        Vtab = sb(es, "Vtab", [128, NTT, 8])
        r_tab = [R() for _ in range(NTT)]

        with ExitStack() as s0:
            cact = sb(s0, "cact", [128, 8, 5])
            r_cact = R()
            dma("sp", cact[:], c5, [], [r_cact], ch="p0a")
            badaT = sb(s0, "badaT", [128, 48])
            r_badaT = R()
            dma("sp", badaT[:], b_adaT, [], [r_badaT], ch="p0a")
            bada5 = sb(s0, "bada5", [5, 6 * D])
            r_bada5 = R()
            dma("sp", bada5[:], b_ada.partition_broadcast(5), [], [r_bada5], ch="p0a")
            lraw = sb(s0, "lraw", [128, 2, 2, 512])
            r_lraw = R()
            dma("sp", lraw[:], lb_raw.partition_broadcast(128), [], [r_lraw], ch="p0a")
            for r_ in (r_cact, r_badaT, r_bada5, r_lraw):
                r_.w = {"d_p0a": p.cnt["d_p0a"]}
            act(cact[:], cact[:], AF.Silu, [r_cact], [r_cact])
            rows = sb(s0, "rows", [5, 6 * D])
            r_rows = R()
            wa = [sb(s0, f"wa{i}", [128, 8, 512]) for i in range(2)]
            r_wa = [R(), R()]
            for g in range(12):
                i = g % 2
                dma("sp", wa[i][:], w_ada[:, g * 512:(g + 1) * 512].rearrange("(kc p) n -> p kc n", p=128),
                    [], [r_wa[i]], ch=f"wa{i}")
                pr, r_pr = bank()
                mm([(pr[0:5, :], [(cact[:, kc, :], wa[i][:, kc, :]) for kc in range(8)])], [r_wa[i], r_cact], [r_pr])
                tt("dve", rows[:, g * 512:(g + 1) * 512], pr[0:5, :], bada5[:, g * 512:(g + 1) * 512], ALU.add,
                   [r_pr, r_bada5], [r_rows])
            for j0 in (8, 32):
                ts("dve", rows[:, j0 * 128:(j0 + 8) * 128], rows[:, j0 * 128:(j0 + 8) * 128], 1.0, None, ALU.add, None,
                   [r_rows], [r_rows])
            pm, r_pm = bank()
            for j in range(48):
                tr(pm[:, j * 5:(j + 1) * 5], rows[0:5, j * 128:(j + 1) * 128], k["ident_f"][0:5, 0:5], [r_rows, kr["ident_f"]], [r_pm])
            cp("dve", modT[:], pm[:, 0:240].rearrange("p (j b) -> p j b", b=5), [r_pm], [r_modT])
            dma("sp", modrows, rows[:], [r_rows], [r_modrows], ch="p0s")
            if debug:
                dma("pool", dbg["mod"], modT[:], [r_modT], [R()], ch="dbg")
            ldiff = sb(s0, "ldiff", [128, 2, 512])
            r_ldiff = R()
            tt("dve", ldiff[:], lraw[:, 0, :, :], lraw[:, 1, :, :], ALU.subtract, [r_lraw], [r_ldiff])
            for dr in range(2):
                act(lbt[:, dr, :], ldiff[:, dr, :], AF.Sigmoid, [r_ldiff], [r_lbt], scale=-1.0)
                act(lbt[:, dr, :], lbt[:, dr, :], AF.Ln, [r_lbt], [r_lbt])

        p.fence()
        pending_casts = []
        for e_ in range(NE):
            pending_casts.append((wgub[e_ * 128:(e_ + 1) * 128, :].rearrange("p (kc f) -> p kc f", kc=8),
                                  w_gu[e_].rearrange("(kc p) f -> p kc f", p=128), r_wgub))
            pending_casts.append((wdnb[e_ * 128:(e_ + 1) * 128, :].rearrange("p (kc f) -> p kc f", kc=8),
                                  w_dn[e_].rearrange("(kc p) f -> p kc f", p=128), r_wdnb))

        def issue_casts(n):
            for _ in range(min(n, len(pending_casts))):
                o_, i_, r_ = pending_casts.pop(0)
                dma("pool", o_, i_, [], [], ch="cast", wacc=[r_])

        with ExitStack() as sm:
            uT = sb(sm, "uT", [128, 8, L], BF16)
            r_uT = R()
            ucT = sb(sm, "ucT", [128, 8, LC], BF16)
            r_ucT = R()
            wg = [sb(sm, f"wg{i}", [128, 8, 512], BF16) for i in range(2)]
            r_wg = [R(), R()]
            wg_i = [0]
            yfT = sb(sm, "yfT", [128, 4, L], BF16)
            r_yfT = R()
            yhT = sb(sm, "yhT", [128, 4, L], BF16)
            r_yhT = R()

            def load_wg(c0, ncols=512):
                i = wg_i[0]
                wg_i[0] = 1 - i
                dma("sp", wg[i][:, :, 0:ncols], winb[:, c0:c0 + ncols].rearrange("(kc p) n -> p kc n", p=128),
                    [r_winb], [r_wg[i]], ch=f"wg{i}")
                return wg[i], r_wg[i]

            for b in range(NB):
                p.fence()
                with ExitStack() as s1:
                    xt = [sb(s1, f"xt{i}", [128, 4, D]) for i in range(2)]
                    r_xt = [R(), R()]

                    def make_uT(src, ntiles, dst, r_dst, col):
                        ngr = (ntiles + 3) // 4
                        for g in range(ngr):
                            i = g % 2
                            nt = min(4, ntiles - g * 4)
                            dma("sp", xt[i][:, 0:nt, :],
                                src[g * 512:g * 512 + nt * 128, :].rearrange("(a p) d -> p a d", p=128),
                                [], [r_xt[i]], ch=f"xt{i}")
                            for kc in range(8):
                                pb, r_pb = bank()
                                for a in range(nt):
                                    tr(pb[:, a * 128:(a + 1) * 128], xt[i][:, a, kc * 128:(kc + 1) * 128], k["ident_f"][:],
                                       [r_xt[i], kr["ident_f"]], [r_pb])
                                act(dst[:, kc, g * 512:g * 512 + nt * 128], pb[:, 0:nt * 128], AF.Identity,
                                    [r_pb, r_modT], [r_dst], bias=modT[:, kc, col:col + 1], scale=modT[:, 8 + kc, col:col + 1])
                    make_uT(x[b], NT, uT, r_uT, b)
                    make_uT(ctx[b], 2, ucT, r_ucT, 4)
                    if b == 0:
                        dbgout("uT", uT[:], [128, 8, L], BF16, [r_uT])

                p.fence()
                with ExitStack() as s3:
                    qT = sb(s3, "qT", [128, 4, L], BF16)
                    r_qT = R()
                    vtm = sb(s3, "vtm", [128, NT, 512], BF16)
                    r_vtm = R()
                    vctm = sb(s3, "vctm", [128, 2, 512], BF16)
                    r_vctm = R()
                    oacc = sb(s3, "oacc", [128, 4, L], BF16)
                    r_oacc = [R() for _ in range(NT)]
                    LF32 = sb(s3, "LF32", [128, 512])
                    LK32 = sb(s3, "LK32", [128, 512])
                    r_LF32, r_LK32 = R(), R()
                    LFh = [sb(s3, f"LFh{d_}", [128, 512], BF16) for d_ in range(2)]
                    LFl = [sb(s3, f"LFl{d_}", [128, 512], BF16) for d_ in range(2)]
                    kb = [sb(s3, f"kb{d_}", [128, 512], BF16) for d_ in range(2)]
                    eqn = [sb(s3, f"eqn{d_}", [128, 512]) for d_ in range(2)]
                    er = sb(s3, "er", [128, 512], BF16)
                    r_eqn = [R(), R()]
                    r_er = R()
                    r_LF = [R(), R()]
                    r_LK = [R(), R()]
                    sg = sb(s3, "sg", [128, 512])
                    sgn = sb(s3, "sgn", [128, 512])
                    r_sg, r_sgn = R(), R()
                    eq = [sb(s3, f"eq{d_}", [128, 512]) for d_ in range(2)]
                    r_eq = [R(), R()]
                    qdec = [sb(s3, f"qdec{d_}", [128, 4, 128], BF16) for d_ in range(2)]
                    kinv = [sb(s3, f"kinv{d_}", [128, 4, 128], BF16) for d_ in range(2)]
                    kend = [sb(s3, f"kend{d_}", [128, 512], BF16) for d_ in range(2)]
                    scm = [sb(s3, f"scm{d_}", [128, 4, 128], BF16) for d_ in range(2)]
                    r_qdec, r_kinv, r_kend, r_scm = [[R(), R()] for _ in range(4)]
                    S = [sb(s3, f"S{d_}", [128, 4, 128]) for d_ in range(2)]
                    Sb = [sb(s3, f"Sb{d_}", [128, 4, 4, 128], BF16) for d_ in range(2)]
                    r_S = [R(), R()]
                    r_Sb = [[R() for _ in range(4)] for _ in range(2)]

                    wq, r_wq = load_wg(512)
                    for tq in range(4):
                        for h in range(4):
                            pb, r_pb = bank()
                            mm([(pb[:], [(wq[:, kc, h * 128:(h + 1) * 128], uT[:, kc, tq * 512:(tq + 1) * 512]) for kc in range(8)])],
                               [r_wq, r_uT], [r_pb])
                            act(qT[:, h, tq * 512:(tq + 1) * 512], pb[:], AF.Silu, [r_pb], [r_qT])
                    wv, r_wv = load_wg(2048)
                    for ti in range(NT):
                        pb, r_pb = bank()
                        mm([(pb[:], [(uT[:, kc, ti * 128:(ti + 1) * 128], wv[:, kc, :]) for kc in range(8)])], [r_wv, r_uT], [r_pb])
                        cp("act", vtm[:, ti, :], pb[:], [r_pb], [r_vtm])
                    for ti in range(2):
                        pb, r_pb = bank()
                        mm([(pb[:], [(ucT[:, kc, ti * 128:(ti + 1) * 128], wv[:, kc, :]) for kc in range(8)])], [r_wv, r_ucT], [r_pb])
                        cp("act", vctm[:, ti, :], pb[:], [r_pb], [r_vctm])
                    wz = [None, None]
                    r_wz = [None, None]
                    wz[0], r_wz[0] = load_wg(1024)
                    wz[1], r_wz[1] = load_wg(1536)

                    for dr in range(2):
                        p.op("dve", lambda e, dr=dr, S=S: e.memset(S[dr][:], 0.0), writes=[r_S[dr]])
                        p.op("dve", lambda e, dr=dr, Sb=Sb: e.memset(Sb[dr][:], 0.0), writes=r_Sb[dr])

                    def gla_step(dr, step, ti, srcT, r_src, vt, r_vt, latent, first_write):
                        tri = k["tri"]
                        issue_casts(1)
                        pz, r_pz = bank()
                        mm([(pz[:], [(srcT[:, kc, ti * 128:(ti + 1) * 128], wz[dr][:, kc, :]) for kc in range(8)])],
                           [r_wz[dr], r_src], [r_pz])
                        act(sg[:], pz[:], AF.Exp, [r_pz], [r_sg])
                        act(sgn[:], sg[:], AF.Ln, [r_sg, r_eps], [r_sgn], bias=epst[:, 2:3], scale=1.0)
                        tt("dve", LK32[:], lbt[:, dr, :], sgn[:], ALU.subtract, [r_sgn, r_lbt], [r_LK32])
                        act(sg[:], LK32[:], AF.Exp, [r_LK32], [r_sg])
                        act(LF32[:], sg[:], AF.Ln, [r_sg, r_eps], [r_LF32], bias=epst[:, 2:3], scale=-1.0)
                        cp("act", LFh[dr][:], LF32[:], [r_LF32], [r_LF[dr]])
                        tt("dve", LFl[dr][:], LF32[:], LFh[dr][:], ALU.subtract, [r_LF32, r_LF[dr]], [r_LF[dr]])
                        cp("dve", kb[dr][:], sg[:], [r_sg], [r_LK[dr]])
                        pq, r_pq = bank()
                        mm([(pq[:, h * 128:(h + 1) * 128], [(LFh[dr][:, h * 128:(h + 1) * 128], tri[:, dr, 0, :]),
                                                           (LFl[dr][:, h * 128:(h + 1) * 128], tri[:, dr, 0, :])]) for h in range(4)],
                           [r_LF[dr], kr["tri"]], [r_pq])
                        act(eq[dr][:], pq[:], AF.Exp, [r_pq], [r_eq[dr]])
                        if latent:
                            act(eqn[dr][:], pq[:], AF.Exp, [r_pq], [r_eqn[dr]], scale=-1.0)
                        pe_, r_pe = bank()
                        mm([(pe_[:], [(tri[:, dr, 1, :], LFh[dr][:]), (tri[:, dr, 1, :], LFl[dr][:])])],
                           [r_LF[dr], kr["tri"]], [r_pe])
                        act(er[:], pe_[:], AF.Exp, [r_pe], [r_er])
                        tt("dve", kend[dr][:], er[:], kb[dr][:], ALU.mult, [r_er, r_LK[dr]], [r_kend[dr]])
                        if latent:
                            pk, r_pk = bank()
                            mm([(pk[:, h * 128:(h + 1) * 128], [(kb[dr][:, h * 128:(h + 1) * 128], k["ident_b"][:])]) for h in range(4)],
                               [r_LK[dr], kr["ident_b"]], [r_pk])
                            tt("dve", kinv[dr][:].rearrange("p h t -> p (h t)"), pk[:], eqn[dr][:], ALU.mult, [r_pk, r_eqn[dr]], [r_kinv[dr]])
                            tt("dve", qdec[dr][:], eq[dr][:].rearrange("p (h t) -> p h t", h=4), qT[:, :, ti * 128:(ti + 1) * 128], ALU.mult,
                               [r_eq[dr], r_qT], [r_qdec[dr]])
                            psc, r_psc = bank()
                            mm([(psc[:, h * 128:(h + 1) * 128], [(kinv[dr][:, h, :], qdec[dr][:, h, :])]) for h in range(4)],
                               [r_kinv[dr], r_qdec[dr]], [r_psc])
                            tt("dve", scm[dr][:], psc[:].rearrange("p (h t) -> p h t", h=4),
                               tri[:, dr, 0, :].unsqueeze(1).to_broadcast([128, 4, 128]), ALU.mult, [r_psc, kr["tri"]], [r_scm[dr]])
                        chunks = (0, 1) if dr == 0 else (1, 0)
                        sl = (step % 2) * 2
                        nsl = ((step + 1) % 2) * 2
                        for ci, ch_ in enumerate(chunks):
                            c0 = ch_ * 64
                            gcol = c0 + 63 if dr == 0 else c0
                            pkv, r_pkv = bank()
                            mm([(pkv[:, h * 128:(h + 1) * 128], [(kend[dr][c0:c0 + 64, h * 128:(h + 1) * 128], vt[c0:c0 + 64, ti, h * 128:(h + 1) * 128])])
                                for h in range(4)], [r_kend[dr], r_vt], [r_pkv])
                            for h in range(4):
                                stt(S[dr][:, h, :], S[dr][:, h, :], eq[dr][:, h * 128 + gcol:h * 128 + gcol + 1], pkv[:, h * 128:(h + 1) * 128],
                                    ALU.mult, ALU.add, [r_S[dr], r_eq[dr], r_pkv], [r_S[dr]])
                            dst = sl + 1 if ci == 0 else nsl
                            cp("act", Sb[dr][:, dst, :, :], S[dr][:], [r_S[dr]], [r_Sb[dr][dst]])
                        if latent:
                            po, r_po = bank()

                            def fn(e, dr=dr, ti=ti, chunks=chunks, sl=sl, po=po, vt=vt, scm=scm, Sb=Sb, qdec=qdec):
                                inst = None
                                for h in range(4):
                                    inst = e.matmul(po[:, h * 128:(h + 1) * 128], vt[:, ti, h * 128:(h + 1) * 128], scm[dr][:, h, :],
                                                    start=True, stop=False)
                                    for ci, ch_ in enumerate(chunks):
                                        c0 = ch_ * 64
                                        inst = e.matmul(po[:, h * 128 + c0:h * 128 + c0 + 64], Sb[dr][:, sl + ci, h, :],
                                                        qdec[dr][:, h, c0:c0 + 64], start=False, stop=(ci == 1))
                                return inst
                            p.op("pe", fn, reads=[r_vt, r_scm[dr], r_qdec[dr], r_Sb[dr][sl], r_Sb[dr][sl + 1]], writes=[r_po])
                            ov = oacc[:, :, ti * 128:(ti + 1) * 128]
                            pov = po[:].rearrange("p (h t) -> p h t", h=4)
                            if first_write:
                                cp("act", ov, pov, [r_po], [r_oacc[ti]])
                            else:
                                tt("dve", ov, pov, ov, ALU.add, [r_po, r_oacc[ti]], [r_oacc[ti]])

                    for i in range(2):
                        gla_step(0, i, i, ucT, r_ucT, vctm, r_vctm, False, False)
                        gla_step(1, i, 1 - i, ucT, r_ucT, vctm, r_vctm, False, False)
                    for i in range(NT):
                        gla_step(0, i + 2, i, uT, r_uT, vtm, r_vtm, True, i < NT // 2)
                        gla_step(1, i + 2, NT - 1 - i, uT, r_uT, vtm, r_vtm, True, i < NT // 2)

                    if b == 0:
                        dbgout("qT", qT[:], [128, 4, L], BF16, [r_qT])
                        dbgout("oacc", oacc[:], [128, 4, L], BF16, r_oacc)
                        dbgout("Sf", S[0][:], [128, 4, 128], F32, [r_S[0]])
                    wgg, r_wgg = load_wg(2560)
                    sq = sb(s3, "sq", [128, 512])
                    r_sq = R()
                    rstd = sb(s3, "rstd", [128, 512])
                    r_rstd = R()
                    sgate = sb(s3, "sgate", [128, 512])
                    r_sgate = R()
                    r_ALL_o = r_oacc
                    for tq in range(4):
                        for h in range(4):
                            osl = oacc[:, h, tq * 512:(tq + 1) * 512]
                            act(sq[:], osl, AF.Square, r_ALL_o[tq * 4:(tq + 1) * 4], [r_sq])
                            pb, r_pb = bank()
                            mm([(pb[:], [(k["ones_f"][:], sq[:])])], [r_sq, kr["ones_f"]], [r_pb])
                            act(rstd[:], pb[:], AF.Sqrt, [r_pb, r_eps], [r_rstd], bias=epst[:, 0:1], scale=1.0 / 128.0)
                            p.op("dve", lambda e, rstd=rstd: e.reciprocal(rstd[:], rstd[:]), reads=[r_rstd], writes=[r_rstd])
                            pg_, r_pg = bank()
                            mm([(pg_[:], [(wgg[:, kc, h * 128:(h + 1) * 128], uT[:, kc, tq * 512:(tq + 1) * 512]) for kc in range(8)])],
                               [r_wgg, r_uT], [r_pg])
                            act(sgate[:], pg_[:], AF.Silu, [r_pg], [r_sgate])
                            tt("dve", rstd[:], rstd[:], osl, ALU.mult, [r_rstd] + r_ALL_o[tq * 4:(tq + 1) * 4], [r_rstd])
                            stt(yhT[:, h, tq * 512:(tq + 1) * 512], rstd[:], normg[:, h:h + 1], sgate[:], ALU.mult, ALU.mult,
                                [r_rstd, r_normg, r_sgate], [r_yhT])

                if b == 0:
                    dbgout("yhT", yhT[:], [128, 4, L], BF16, [r_yhT])
                p.fence()
                with ExitStack() as s2:
                    pftm = sb(s2, "pftm", [128, NT, 512], BF16)
                    r_pftm = R()
                    dft = [sb(s2, f"dft{i}", [128, NT, 512], BF16) for i in range(2)]
                    r_dft = [R(), R()]
                    AB = sb(s2, "AB", [128, 2, 4, 512], BF16)
                    r_AB = [R(), R()]
                    wf, r_wf = load_wg(0)
                    for ti in range(NT):
                        pb, r_pb = bank()
                        mm([(pb[:], [(uT[:, kc, ti * 128:(ti + 1) * 128], wf[:, kc, :]) for kc in range(8)])], [r_wf, r_uT], [r_pb])
                        cp("act" if ti % 2 else "dve", pftm[:, ti, :], pb[:], [r_pb], [r_pftm])
                    srcs = (cd["ctok"], cd["stok"])
                    for tq in range(4):
                        for cs in range(2):
                            dma("sp", dft[cs][:], srcs[cs][:, tq * 512:(tq + 1) * 512].rearrange("(ti p) t -> p ti t", p=128),
                                [], [r_dft[cs]], ch=f"dft{cs}")
                            for g in range(4):
                                pb, r_pb = bank()
                                mm([(pb[:], [(pftm[:, ti, g * 128:(g + 1) * 128], dft[cs][:, ti, :]) for ti in range(NT)])],
                                   [r_pftm, r_dft[cs]], [r_pb])
                                cp("act" if g % 2 else "dve", AB[:, cs, g, :], pb[:], [r_pb], [r_AB[cs]])
                        for g in range(4):
                            pb, r_pb = bank()
                            mm([(pb[:], [(k["cch"][:], AB[:, 0, g, :]), (k["schn"][:], AB[:, 1, g, :])])],
                               [r_AB[0], r_AB[1], kr["cch"], kr["schn"]], [r_pb])
                            cp("act" if g % 2 else "dve", yfT[:, g, tq * 512:(tq + 1) * 512], pb[:], [r_pb], [r_yfT])

                if b == 0:
                    dbgout("yfT", yfT[:], [128, 4, L], BF16, [r_yfT])
                p.fence()
                with ExitStack() as s4:
                    wfo_t = sb(s4, "wfo_t", [128, 4, D], BF16)
                    who_t = sb(s4, "who_t", [128, 4, D], BF16)
                    wo_t = sb(s4, "wo_t", [128, 8, D], BF16)
                    r_wfo_t, r_who_t, r_wo_t = R(), R(), R()
                    dma("sp", wfo_t[:], wfob.rearrange("(kc p) n -> p kc n", p=128), [r_wfob], [r_wfo_t], ch="wm0")
                    dma("sp", who_t[:], whob.rearrange("(kc p) n -> p kc n", p=128), [r_whob], [r_who_t], ch="wm1")
                    dma("sp", wo_t[:], wob.rearrange("(kc p) n -> p kc n", p=128), [r_wob], [r_wo_t], ch="wm2")
                    bc = sb(s4, "bc", [128, 5, D])
                    r_bc = R()
                    for i, j0 in enumerate((16, 32, 24)):
                        dma("sp", bc[:, i, :], modrows[b:b + 1, j0 * 128:(j0 + 8) * 128].partition_broadcast(128), [r_modrows], [r_bc], ch="bc")
                    dma("sp", bc[:, 3, :], ln1_g.partition_broadcast(128), [], [r_bc], ch="bc")
                    dma("sp", bc[:, 4, :], ln1_b.partition_broadcast(128), [], [r_bc], ch="bc")
                    mTs = [sb(s4, f"mT{i}", [128, 8, 512], BF16) for i in range(2)]
                    r_mTs = [R(), R()]
                    sgf = sb(s4, "sgf", [128, 512])
                    sgh = sb(s4, "sgh", [128, 512])
                    r_sgf, r_sgh = R(), R()
                    xr = [sb(s4, f"xr{i}", [128, D]) for i in range(2)]
                    r_xr = [R(), R()]
                    tbuf = sb(s4, "tbuf", [128, D])
                    r_tbuf = R()
                    x1t = sb(s4, "x1t", [128, D])
                    r_x1t = R()
                    u2t = sb(s4, "u2t", [128, D])
                    r_u2t = R()
                    u2bt = sb(s4, "u2bt", [128, D], BF16)
                    r_u2bt = R()
                    u2T = sb(s4, "u2T", [128, 8, 128])
                    r_u2T = R()
                    stats = sb(s4, "stats", [128, 2, 6])
                    mv = sb(s4, "mv", [128, 2])
                    nmr = sb(s4, "nmr", [128, 2])
                    r_stats, r_mv, r_nmr = R(), R(), R()

                    def layer_norm(src, r_src, dst, r_dst, gidx, bidx, bct, r_bct):
                        for hf in range(2):
                            p.op("dve", lambda e, hf=hf, stats=stats, src=src: e.bn_stats(stats[:, hf, :], src[:, hf * 512:(hf + 1) * 512]), reads=[r_src], writes=[r_stats])
                        p.op("dve", lambda e, mv=mv, stats=stats: e.bn_aggr(mv[:], stats[:].rearrange("p a s -> p (a s)")), reads=[r_stats], writes=[r_mv])
                        act(nmr[:, 0:1], mv[:, 1:2], AF.Sqrt, [r_mv, r_eps], [r_nmr], bias=epst[:, 1:2], scale=1.0)
                        p.op("dve", lambda e, nmr=nmr: e.reciprocal(nmr[:, 0:1], nmr[:, 0:1]), reads=[r_nmr], writes=[r_nmr])
                        stt(dst[:], src[:], mv[:, 0:1], bct[:, gidx, :], ALU.subtract, ALU.mult, [r_src, r_mv, r_bct], [r_dst])
                        stt(dst[:], dst[:], nmr[:, 0:1], bct[:, bidx, :], ALU.mult, ALU.add, [r_dst, r_nmr, r_bct], [r_dst])

                    def gates_j(tq, j):
                        wgj = [None, None]
                        r_wgj = [None, None]
                        for br in range(2):
                            wgj[br], r_wgj[br] = load_wg(3072 + br * 1024 + j * 128, 128)
                        for br, (sgt, r_sgt, yT, r_yT, wpt, r_wpt) in enumerate(((sgf, r_sgf, yfT, r_yfT, wfo_t, r_wfo_t),
                                                                               (sgh, r_sgh, yhT, r_yhT, who_t, r_who_t))):
                            pb, r_pb = bank()
                            mm([(pb[:], [(wgj[br][:, kc, 0:128], uT[:, kc, tq * 512:(tq + 1) * 512]) for kc in range(8)])],
                               [r_wgj[br], r_uT], [r_pb])
                            act(sgt[:], pb[:], AF.Sigmoid, [r_pb], [r_sgt])
                            pb2, r_pb2 = bank()
                            mm([(pb2[:], [(wpt[:, kc, j * 128:(j + 1) * 128], yT[:, kc, tq * 512:(tq + 1) * 512]) for kc in range(4)])],
                               [r_wpt, r_yT], [r_pb2])
                            tt("dve", sgt[:], sgt[:], pb2[:], ALU.mult, [r_sgt, r_pb2], [r_sgt])
                        tt("dve", mTs[tq % 2][:, j, :], sgf[:], sgh[:], ALU.add, [r_sgf, r_sgh], [r_mTs[tq % 2]])
                    def ln_tile(tq, a):
                        ti = tq * 4 + a
                        gt = b * NT + ti
                        i = ti % 2
                        dma("sp", xr[i][:], x[b, ti * 128:(ti + 1) * 128, :], [], [r_xr[i]], ch=f"xr{i}")
                        for hf in range(2):
                            pb, r_pb = bank()
                            mm([(pb[:], [(mTs[tq % 2][:, kc, a * 128:(a + 1) * 128], wo_t[:, kc, hf * 512:(hf + 1) * 512]) for kc in range(8)])],
                               [r_mTs[tq % 2], r_wo_t], [r_pb])
                            tt("dve", tbuf[:, hf * 512:(hf + 1) * 512], pb[:], bc[:, 0, hf * 512:(hf + 1) * 512], ALU.mult, [r_pb, r_bc], [r_tbuf])
                        stt(tbuf[:], xr[i][:], ALPHA, tbuf[:], ALU.mult, ALU.add, [r_xr[i], r_tbuf], [r_tbuf])
                        layer_norm(tbuf, r_tbuf, x1t, r_x1t, 3, 4, bc, r_bc)
                        dma("sp", x1s[gt * 128:(gt + 1) * 128, :], x1t[:], [r_x1t], [], ch="x1s", wacc=[r_x1s])
                        if debug:
                            dma("pool", dbg["x1"][gt * 128:(gt + 1) * 128, :], x1t[:], [r_x1t], [R()], ch="dbg")
                        tt("dve", u2t[:], x1t[:], bc[:, 1, :], ALU.mult, [r_x1t, r_bc], [r_u2t])
                        tt("dve", u2t[:], u2t[:], bc[:, 2, :], ALU.add, [r_u2t, r_bc], [r_u2t])
                        cp("act", u2bt[:], u2t[:], [r_u2t], [r_u2bt])
                        dma("sp", u2b[gt * 128:(gt + 1) * 128, :], u2bt[:], [r_u2bt], [], ch="u2b", wacc=[r_u2b])
                        for hf in range(2):
                            pb, r_pb = bank()
                            for kk in range(4):
                                kc = hf * 4 + kk
                                tr(pb[:, kk * 128:(kk + 1) * 128], u2t[:, kc * 128:(kc + 1) * 128], k["ident_f"][:], [r_u2t, kr["ident_f"]], [r_pb])
                            cp("act", u2T[:, hf * 4:(hf + 1) * 4, :].rearrange("p a t -> p (a t)"), pb[:], [r_pb], [r_u2T])
                        pb, r_pb = bank()
                        mm([(pb[:, 0:NE], [(u2T[:, kc, :], wr[:, kc, :]) for kc in range(8)])], [r_u2T, r_wr], [r_pb])
                        tt("dve", Ltab[:, gt, :], pb[:, 0:NE], brt[:], ALU.add, [r_pb, r_brt], [r_tab[gt]])
                        p.op("dve", lambda e, gt=gt: e.max(Vtab[:, gt, :], Ltab[:, gt, :]), reads=[r_tab[gt]], writes=[r_tab[gt]])

                    for j in range(8):
                        gates_j(0, j)
                    for tq in range(4):
                        for a in range(4):
                            if tq + 1 < 4:
                                gates_j(tq + 1, 2 * a)
                                gates_j(tq + 1, 2 * a + 1)
                            ln_tile(tq, a)
        issue_casts(len(pending_casts))
        p.fence()
        dbgout("Ltab", Ltab[:], [128, NTT, NE], F32, r_tab)
        dbgout("Vtab", Vtab[:], [128, NTT, 8], F32, r_tab)
        desti = sb(es, "desti", [128, 4, NTT], I32)
        r_desti = R()
        Wtab = sb(es, "Wtab", [128, NTT, 4])
        r_Wtab = R()
        idxw = sb(es, "idxw", [128, NBLK], I32)
        idxe = sb(es, "idxe", [128, NBLK], I32)
        r_idx = R()
        ALLTAB = r_tab

        with ExitStack() as sd:
            pos = sb(sd, "pos", [128, NTT, NE])
            r_pos = R()
            Mtab = sb(sd, "Mtab", [128, NTT, NE])
            r_M = R()
            tt("dve", Mtab[:], Ltab[:], Vtab[:, :, 3:4].to_broadcast([128, NTT, NE]), ALU.is_ge, r_tab, [r_M])
            k["le_mask"] = sb(sd, "c_le_mask", [128, NE * NE])
            kr["le_mask"] = R()
            dma("sp", k["le_mask"][:], cd["le_mask"], [], [kr["le_mask"]], ch="const")
            run = sb(sd, "run", [128, NE])
            r_run = R()
            p.op("dve", lambda e: e.memset(run[:], 0.0), writes=[r_run])
            for gt in range(NTT):
                pb, r_pb = bank()
                mm([(pb[:, 0:NE], [(k["ltri"][:], Mtab[:, gt, :])]), (pb[:, NE:2 * NE], [(k["ones_f"][:], Mtab[:, gt, :])])],
                   [r_M, kr["ltri"], kr["ones_f"]], [r_pb])
                tt("dve", pos[:, gt, :], pb[:, 0:NE], run[:], ALU.add, [r_pb, r_run], [r_pos])
                tt("dve", run[:], pb[:, NE:2 * NE], run[:], ALU.add, [r_pb, r_run], [r_run])
            big = sb(sd, "big", [128, NE * NBLK])
            r_big = R()
            nblk = sb(sd, "nblk", [128, NE])
            padded = sb(sd, "padded", [128, NE])
            pend = sb(sd, "pend", [128, NE])
            pstart = sb(sd, "pstart", [128, NE])
            r_sm = R()
            tt("dve", big[:].rearrange("p (e j) -> p e j", e=NE), run[:].unsqueeze(2).to_broadcast([128, NE, NBLK]),
               k["blk_thr"][:].unsqueeze(1).to_broadcast([128, NE, NBLK]), ALU.is_gt, [r_run, kr["blk_thr"]], [r_big])
            p.op("dve", lambda e: e.reduce_sum(nblk[:], big[:].rearrange("p (e j) -> p e j", e=NE), axis=AX.X), reads=[r_big], writes=[r_sm])
            ts("dve", padded[:], nblk[:], float(BLK), None, ALU.mult, None, [r_sm], [r_sm])
            tt("dve", big[:, 0:NE * NE].rearrange("p (e f) -> p e f", e=NE), padded[:].unsqueeze(1).to_broadcast([128, NE, NE]),
               k["le_mask"][:].rearrange("p (e f) -> p e f", e=NE), ALU.mult, [r_sm, kr["le_mask"], r_big], [r_big])
            p.op("dve", lambda e: e.reduce_sum(pend[:], big[:, 0:NE * NE].rearrange("p (e f) -> p e f", e=NE), axis=AX.X), reads=[r_big], writes=[r_sm])
            tt("dve", pstart[:], pend[:], padded[:], ALU.subtract, [r_sm], [r_sm])
            tt("dve", big[:].rearrange("p (j e) -> p j e", e=NE), pend[:].unsqueeze(1).to_broadcast([128, NBLK, NE]),
               k["blk_thr"][:].unsqueeze(2).to_broadcast([128, NBLK, NE]), ALU.is_le, [r_sm, kr["blk_thr"], r_big], [r_big])
            bef = sb(sd, "bef", [128, NBLK])
            r_bef = R()
            p.op("dve", lambda e: e.reduce_sum(bef[:], big[:].rearrange("p (j e) -> p j e", e=NE), axis=AX.X), reads=[r_big], writes=[r_bef])
            ts("dve", bef[:], bef[:], float(NE - 1), None, ALU.min, None, [r_bef], [r_bef])
            cp("dve", idxe[:], bef[:], [r_bef], [r_idx])
            ts("dve", bef[:], bef[:], 128.0, k["iota_p"][:, 0:1], ALU.mult, ALU.add, [r_bef, kr["iota_p"]], [r_bef])
            cp("dve", idxw[:], bef[:], [r_bef], [r_idx])
            tt("dve", pos[:], pos[:], pstart[:].unsqueeze(1).to_broadcast([128, NTT, NE]), ALU.add, [r_pos, r_sm], [r_pos])
            oh = sb(sd, "oh", [128, NTT, NE])
            r_oh = R()
            destf = sb(sd, "destf", [128, 4, NTT])
            r_destf = R()
            for kk in range(4):
                tt("dve", oh[:], Ltab[:], Vtab[:, :, kk:kk + 1].to_broadcast([128, NTT, NE]), ALU.is_equal, ALLTAB, [r_oh])
                tt("dve", oh[:], oh[:], pos[:], ALU.mult, [r_oh, r_pos], [r_oh])
                p.op("dve", lambda e, kk=kk: e.reduce_sum(destf[:, kk, :], oh[:], axis=AX.X), reads=[r_oh], writes=[r_destf])
            cp("dve", desti[:], destf[:], [r_destf], [r_desti])
            ew = sb(sd, "ew", [128, NTT, 4])
            r_ew = R()
            ssum = sb(sd, "ssum", [128, NTT])
            r_ssum = R()
            tt("dve", ew[:], Vtab[:, :, 0:4], Vtab[:, :, 0:1].to_broadcast([128, NTT, 4]), ALU.subtract, ALLTAB, [r_ew])
            act(ew[:], ew[:], AF.Exp, [r_ew], [r_ew])
            p.op("dve", lambda e: e.reduce_sum(ssum[:], ew[:], axis=AX.X), reads=[r_ew], writes=[r_ssum])
            p.op("dve", lambda e: e.reciprocal(ssum[:], ssum[:]), reads=[r_ssum], writes=[r_ssum])
            tt("dve", Wtab[:], ew[:], ssum[:].unsqueeze(2).to_broadcast([128, NTT, 4]), ALU.mult, [r_ew, r_ssum], [r_Wtab])
            ub = [sb(sd, f"ub{i}", [128, D], BF16) for i in range(2)]
            r_ub = [R(), R()]
            for gt in range(NTT):
                i = gt % 2
                dma("sp", ub[i][:], u2b[gt * 128:(gt + 1) * 128, :], [r_u2b], [r_ub[i]], ch=f"ub{i}")
                for kk in range(4):
                    p.op("pool", lambda e, i=i, kk=kk, gt=gt: e.indirect_dma_start(
                        out=xs, out_offset=bass.IndirectOffsetOnAxis(ap=desti[:, kk, gt:gt + 1], axis=0), in_=ub[i][:], in_offset=None),
                        reads=[r_ub[i], r_desti, r_zf], wacc=[r_xs], ch=f"sc{i}")

        dbgout("desti", desti[:], [128, 4, NTT], I32, [r_desti])
        dbgout("Wtab", Wtab[:], [128, NTT, 4], F32, [r_Wtab])
        dbgout("idxw", idxw[:], [128, NBLK], I32, [r_idx])
        p.fence()
        with ExitStack() as se:
            wgu_t = [sb(se, f"wgu_t{i}", [128, 8 * 2 * D], BF16) for i in range(2)]
            wdn_t = [sb(se, f"wdn_t{i}", [128, 8 * D], BF16) for i in range(2)]
            bgu_t = [sb(se, f"bgu_t{i}", [128, 16]) for i in range(2)]
            bdn_t = [sb(se, f"bdn_t{i}", [128, D]) for i in range(2)]
            r_wt = [R(), R()]
            xrows = [sb(se, f"xrows{i}", [128, 4, D], BF16) for i in range(2)]
            r_xrows = [R(), R()]
            xsT = [sb(se, f"xsT{i}", [128, 8, BLK], BF16) for i in range(2)]
            r_xsT = [R(), R()]
            actT = sb(se, "actT", [128, 8, BLK], BF16)
            r_actT = [R() for _ in range(8)]
            gc = [sb(se, f"gc{i}", [128, BLK]) for i in range(2)]
            sgm = [sb(se, f"sgm{i}", [128, BLK]) for i in range(2)]
            uu = [sb(se, f"uu{i}", [128, BLK]) for i in range(2)]
            r_gc, r_sgm, r_uu = [R(), R()], [R(), R()], [R(), R()]
            yst = [sb(se, f"yst{i}", [128, D]) for i in range(2)]
            r_yst = [R(), R()]

            def load_block(j):
                i = j % 2
                def g(dst, src, idx):
                    p.op("pool", lambda e: e.indirect_dma_start(out=dst, out_offset=None, in_=src,
                                                               in_offset=bass.IndirectOffsetOnAxis(ap=idx, axis=0)),
                         reads=[r_idx, r_wgub, r_wdnb], wacc=[r_wt[i]], ch=f"wt{i}")
                g(wgu_t[i][:], wgub, idxw[:, j:j + 1])
                g(wdn_t[i][:], wdnb, idxw[:, j:j + 1])
                g(bgu_t[i][:], bguT, idxw[:, j:j + 1])
                g(bdn_t[i][:], b_dn, idxe[:, j:j + 1])
                dma("sp", xrows[i][:], xs[j * BLK:(j + 1) * BLK, :].rearrange("(a p) d -> p a d", p=128), [r_xs], [r_xrows[i]], ch=f"xrows{i}")

            def transposes(j):
                i = j % 2
                for kc in range(8):
                    pb, r_pb = bank()
                    mm([(pb[:, a * 128:(a + 1) * 128], [(xrows[i][:, a, kc * 128:(kc + 1) * 128], k["ident_b"][:])]) for a in range(4)],
                       [r_xrows[i], kr["ident_b"]], [r_pb])
                    cp("act" if kc % 2 else "dve", xsT[i][:, kc, :], pb[:], [r_pb], [r_xsT[i]])

            load_block(0)
            transposes(0)
            for j in range(NBLK):
                i = j % 2
                if j + 1 < NBLK:
                    load_block(j + 1)
                for fj in range(8):
                    q_ = fj % 2
                    pg_, r_pg = bank()
                    mm([(pg_[:], [(wgu_t[i][:, kc * 2048 + fj * 128:kc * 2048 + (fj + 1) * 128], xsT[i][:, kc, :]) for kc in range(8)])],
                       [r_wt[i], r_xsT[i]], [r_pg])
                    pu_, r_pu = bank()
                    mm([(pu_[:], [(wgu_t[i][:, kc * 2048 + 1024 + fj * 128:kc * 2048 + 1024 + (fj + 1) * 128], xsT[i][:, kc, :]) for kc in range(8)])],
                       [r_wt[i], r_xsT[i]], [r_pu])
                    ts("dve", gc[q_][:], pg_[:], bgu_t[i][:, fj:fj + 1], 7.0, ALU.add, ALU.min, [r_pg, r_wt[i]], [r_gc[q_]])
                    act(sgm[q_][:], gc[q_][:], AF.Sigmoid, [r_gc[q_]], [r_sgm[q_]], scale=1.702)
                    act(uu[q_][:], pu_[:], AF.Identity, [r_pu, r_wt[i]], [r_uu[q_]], bias=bgu_t[i][:, 8 + fj:9 + fj], scale=1.0)
                    ts("dve", uu[q_][:], uu[q_][:], 7.0, -7.0, ALU.min, ALU.max, [r_uu[q_]], [r_uu[q_]])
                    tt("pool", gc[q_][:], gc[q_][:], sgm[q_][:], ALU.mult, [r_gc[q_], r_sgm[q_]], [r_gc[q_]])
                    stt(actT[:, fj, :], uu[q_][:], 1.0, gc[q_][:], ALU.add, ALU.mult, [r_gc[q_], r_uu[q_]], [r_actT[fj]])
                if j + 1 < NBLK:
                    transposes(j + 1)
                for a in range(4):
                    q_ = a % 2
                    for hf in range(2):
                        pb, r_pb = bank()
                        mm([(pb[:], [(actT[:, fc, a * 128:(a + 1) * 128], wdn_t[i][:, fc * 1024 + hf * 512:fc * 1024 + (hf + 1) * 512]) for fc in range(8)])],
                           r_actT + [r_wt[i]], [r_pb])
                        tt("dve", yst[q_][:, hf * 512:(hf + 1) * 512], pb[:], bdn_t[i][:, hf * 512:(hf + 1) * 512], ALU.add, [r_pb, r_wt[i]], [r_yst[q_]])
                    dma("sp", ys[j * BLK + a * 128:j * BLK + (a + 1) * 128, :], yst[q_][:], [r_yst[q_]], [], ch=f"yst{q_}", wacc=[r_ys])

        p.fence()
        with ExitStack() as sc_:
            bc2 = sb(sc_, "bc2", [128, NB + 2, D])
            r_bc2 = R()
            for b in range(NB):
                dma("sp", bc2[:, b, :], modrows[b:b + 1, 40 * 128:48 * 128].partition_broadcast(128), [r_modrows], [r_bc2], ch="bc2")
            dma("sp", bc2[:, NB, :], ln2_g.partition_broadcast(128), [], [r_bc2], ch="bc2")
            dma("sp", bc2[:, NB + 1, :], ln2_b.partition_broadcast(128), [], [r_bc2], ch="bc2")
            yg = [sb(sc_, f"yg{i}", [128, 4, D]) for i in range(2)]
            r_yg = [R(), R()]
            x1r = [sb(sc_, f"x1r{i}", [128, D]) for i in range(2)]
            r_x1r = [R(), R()]
            ffs = [sb(sc_, f"ff{i}", [128, D]) for i in range(2)]
            r_ffs = [R(), R()]
            ot = [sb(sc_, f"ot{i}", [128, D]) for i in range(2)]
            r_ot = [R(), R()]
            stats2s = [sb(sc_, f"stats2{i}", [128, 2, 6]) for i in range(2)]
            mv2s = [sb(sc_, f"mv2{i}", [128, 2]) for i in range(2)]
            nmr2s = [sb(sc_, f"nmr2{i}", [128, 2]) for i in range(2)]
            r_stats2s, r_mv2s, r_nmr2s = [R(), R()], [R(), R()], [R(), R()]
            r_out = R()
            outf = out.rearrange("b l d -> (b l) d")

            def load_c(gt):
                i = gt % 2
                for kk in range(4):
                    p.op("pool", lambda e, kk=kk: e.indirect_dma_start(out=yg[i][:, kk, :], out_offset=None, in_=ys,
                                                                      in_offset=bass.IndirectOffsetOnAxis(ap=desti[:, kk, gt:gt + 1], axis=0)),
                         reads=[r_desti, r_ys], wacc=[r_yg[i]], ch=f"yg{i}")
                dma("sp", x1r[i][:], x1s[gt * 128:(gt + 1) * 128, :], [r_x1s], [r_x1r[i]], ch=f"x1r{i}")

            load_c(0)
            for gt in range(NTT):
                i = gt % 2
                b = gt // NT
                if gt + 1 < NTT:
                    load_c(gt + 1)
                ff, r_ff = ffs[i], r_ffs[i]
                stats2, mv2, nmr2 = stats2s[i], mv2s[i], nmr2s[i]
                r_stats2, r_mv2, r_nmr2 = r_stats2s[i], r_mv2s[i], r_nmr2s[i]
                act(ff[:], yg[i][:, 0, :], AF.Identity, [r_yg[i], r_Wtab], [r_ff], scale=Wtab[:, gt, 0:1])
                for kk in range(1, 4):
                    stt(ff[:], yg[i][:, kk, :], Wtab[:, gt, kk:kk + 1], ff[:], ALU.mult, ALU.add, [r_yg[i], r_Wtab, r_ff], [r_ff])
                tt("dve", ff[:], ff[:], bc2[:, b, :], ALU.mult, [r_ff, r_bc2], [r_ff])
                stt(ff[:], x1r[i][:], ALPHA, ff[:], ALU.mult, ALU.add, [r_x1r[i], r_ff], [r_ff])
                for hf in range(2):
                    p.op("dve", lambda e, hf=hf, stats2=stats2, ff=ff: e.bn_stats(stats2[:, hf, :], ff[:, hf * 512:(hf + 1) * 512]), reads=[r_ff], writes=[r_stats2])
                p.op("dve", lambda e, mv2=mv2, stats2=stats2: e.bn_aggr(mv2[:], stats2[:].rearrange("p a s -> p (a s)")), reads=[r_stats2], writes=[r_mv2])
                act(nmr2[:, 0:1], mv2[:, 1:2], AF.Sqrt, [r_mv2, r_eps], [r_nmr2], bias=epst[:, 1:2], scale=1.0)
                p.op("dve", lambda e, nmr2=nmr2: e.reciprocal(nmr2[:, 0:1], nmr2[:, 0:1]), reads=[r_nmr2], writes=[r_nmr2])
                stt(nmr2[:, 1:2], mv2[:, 0:1], -1.0, nmr2[:, 0:1], ALU.mult, ALU.mult, [r_mv2, r_nmr2], [r_nmr2])
                act(ot[i][:], ff[:], AF.Identity, [r_ff, r_nmr2], [r_ot[i]], bias=nmr2[:, 1:2], scale=nmr2[:, 0:1])
                tt("pool", ot[i][:], ot[i][:], bc2[:, NB, :], ALU.mult, [r_ot[i], r_bc2], [r_ot[i]])
                tt("dve", ot[i][:], ot[i][:], bc2[:, NB + 1, :], ALU.add, [r_ot[i], r_bc2], [r_ot[i]])
                dma("sp", outf[gt * 128:(gt + 1) * 128, :], ot[i][:], [r_ot[i]], [], ch=f"ot{i}", wacc=[r_out])
            p.final_wait("sp", [r_out])
        p.emit()
    return nc


_NC = {}


def kernel(**inputs):
    debug = bool(inputs.pop("_debug", False))
    if debug not in _NC:
        _NC[debug] = build(debug)
    nc = _NC[debug]
    f = lambda a: np.ascontiguousarray(np.asarray(a, dtype=np.float32))
    consts = _consts()
    shared = {
        "w_ada": f(inputs["w_ada"][0]),
        "b_adaT": f(np.asarray(inputs["b_ada"][0]).reshape(48, 128).T),
        "b_ada": f(inputs["b_ada"]),
        "w_in": f(inputs["w_in"][0]),
        "lb_raw": f(inputs["lb_raw"]),
        "normgT": f(np.asarray(inputs["hg_norm_g"][0]).reshape(4, 128).T),
        "w_four_out": f(inputs["w_four_out"][0]),
        "w_hg_out": f(inputs["w_hg_out"][0]),
        "w_o": f(inputs["w_o"][0]),
        "ln1_g": f(inputs["ln1_g"]), "ln1_b": f(inputs["ln1_b"]),
        "w_router": f(inputs["w_router"][0]), "b_router": f(inputs["b_router"]),
        "w_gate_up": f(inputs["w_gate_up"][0]), "b_guT": f(np.asarray(inputs["b_gate_up"][0]).reshape(NE, 16, 128).transpose(0, 2, 1).reshape(NE * 128, 16)),
        "w_down": f(inputs["w_down"][0]), "b_down": f(inputs["b_down"][0]),
        "ln2_g": f(inputs["ln2_g"]), "ln2_b": f(inputs["ln2_b"]),
    }
    for kname, v in consts.items():
        shared["k_" + kname] = v
    x = np.asarray(inputs["x"], dtype=np.float32)
    ctx = np.asarray(inputs["ctx"], dtype=np.float32)
    c = np.asarray(inputs["c"], dtype=np.float32)
    c_ctx = np.asarray(inputs["c_ctx"], dtype=np.float32)
    in_maps = []
    for core in range(NCORES):
        sl = slice(core * NB, (core + 1) * NB)
        c5 = np.concatenate([c[sl], c_ctx[None, :]], axis=0)
        c5 = np.ascontiguousarray(c5.reshape(5, 8, 128).transpose(2, 1, 0))
        m = dict(shared)
        m["x"] = np.ascontiguousarray(x[sl])
        m["ctx"] = np.ascontiguousarray(ctx[sl])
        m["c5"] = c5
        in_maps.append(m)
    res = run_bass_kernel_spmd(nc, in_maps, core_ids=list(range(NCORES)))
    outs = [np.asarray(r["out"], dtype=np.float32) for r in res.results]
    full = np.concatenate(outs, axis=0)
    if debug:
        return full, res.results
    return full
```

```python
import numpy as np
import ml_dtypes
import concourse.bass as bass
import concourse.mybir as mybir
from contextlib import ExitStack
from concourse.bass_utils import run_bass_kernel_spmd

F32 = mybir.dt.float32
BF16 = mybir.dt.bfloat16
I32 = mybir.dt.int32
AF = mybir.ActivationFunctionType
ALU = mybir.AluOpType
AX = mybir.AxisListType

NCORES = 8
NB = 4
L = 2048
LC = 256
D = 1024
NT = L // 128
NTOK = NB * L
NTT = NTOK // 128
NE = 32
BLK = 512
NBLK = NTOK * 4 // BLK + NE
NROWS = NBLK * BLK
ALPHA = 2.0 ** 0.25
LN_EPS = 1e-5
RMS_EPS = 1e-6


class R:
    __slots__ = ("name", "w", "r")

    def __init__(self, name=""):
        self.name = name
        self.w = {}
        self.r = {}


class Prog:
    ENG = ("pe", "act", "dve", "pool", "sp")

    def __init__(self, nc, es):
        self.nc = nc
        self.es = es
        self.q = {e: [] for e in self.ENG}
        self.semh = {}
        self.cnt = {}
        self.seen = {e: {} for e in self.ENG}
        for e in self.ENG:
            self._sem("c_" + e)

    def _sem(self, name):
        if name not in self.semh:
            self.semh[name] = self.es.enter_context(self.nc.semaphore(name))
            self.cnt[name] = 0
        return self.semh[name]

    def op(self, eng, fn, reads=(), writes=(), ch=None, wacc=()):
        waits = {}

        def merge(d):
            for s, v in d.items():
                if waits.get(s, 0) < v:
                    waits[s] = v
        for r in reads:
            merge(r.w)
        for r in writes:
            merge(r.w)
            merge(r.r)
        for r in wacc:
            merge(r.r)
        own = "c_" + eng
        seen = self.seen[eng]
        wl = []
        for s, v in waits.items():
            if s == own and eng == "pe":
                continue
            if seen.get(s, 0) >= v:
                continue
            seen[s] = v
            wl.append((s, v))
        if ch is None:
            sname, inc = own, 1
        else:
            sname, inc = "d_" + ch, 16
            self._sem(sname)
        self.cnt[sname] += inc
        val = self.cnt[sname]
        self.q[eng].append((wl, fn, sname, inc))
        for r in reads:
            if r.r.get(sname, 0) < val:
                r.r[sname] = val
        for r in writes:
            r.w = {sname: val}
            r.r = {}
        for r in wacc:
            r.w[sname] = val

    def fence(self):
        snap = dict(self.cnt)
        for eng in self.ENG:
            wl = []
            seen = self.seen[eng]
            for s, v in snap.items():
                if v == 0 or seen.get(s, 0) >= v:
                    continue
                if s == "c_" + eng or s in ("d_cast", "d_dbg", "d_zf"):
                    continue
                seen[s] = v
                wl.append((s, v))
            if wl:
                self.q[eng].append((wl, None, None, 0))

    def final_wait(self, eng, resources):
        waits = {}
        for r in resources:
            for s, v in list(r.w.items()) + list(r.r.items()):
                if waits.get(s, 0) < v:
                    waits[s] = v
        self.q[eng].append((list(waits.items()), None, None, 0))

    def emit(self):
        nc = self.nc
        with nc.Block() as block:
            def replay(name, engobj):
                for wl, fn, sname, inc in self.q[name]:
                    for s, v in wl:
                        engobj.wait_ge(self.semh[s], v)
                    if fn is not None:
                        fn(engobj).then_inc(self.semh[sname], inc)

            @block.tensor
            def _(e):
                replay("pe", e)

            @block.scalar
            def _(e):
                replay("act", e)

            @block.vector
            def _(e):
                replay("dve", e)

            @block.gpsimd
            def _(e):
                replay("pool", e)

            @block.sync
            def _(e):
                replay("sp", e)


def _consts():
    c = {}
    c["ident_f"] = np.eye(128, dtype=np.float32)
    c["ident_b"] = np.eye(128, dtype=np.float32).astype(ml_dtypes.bfloat16)
    t = np.arange(128)
    same = (t[:, None] // 64) == (t[None, :] // 64)
    inc_f = (same & (t[:, None] <= t[None, :])).astype(np.float32)
    su_f = (same & (t[:, None] > t[None, :])).astype(np.float32)
    c["tri"] = np.stack([np.stack([inc_f, su_f, -inc_f]), np.stack([inc_f.T, su_f.T, -inc_f.T])]).astype(np.float32)
    c["tri"] = np.ascontiguousarray(c["tri"].transpose(2, 0, 1, 3)).astype(ml_dtypes.bfloat16)
    c["ones_f"] = np.ones((128, 128), np.float32)
    tt = np.arange(L)
    r, w = tt // 64, tt % 64
    ph = (np.outer(r, r) / 32.0 + np.outer(w, w) / 64.0) * 2 * np.pi
    c["ctok"] = (np.cos(ph) / np.sqrt(L)).astype(ml_dtypes.bfloat16)
    c["stok"] = (np.sin(ph) / np.sqrt(L)).astype(ml_dtypes.bfloat16)
    cc = np.arange(128)
    phc = np.outer(cc, cc) * 2 * np.pi / 128.0
    c["cch"] = (np.cos(phc) / np.sqrt(128)).astype(ml_dtypes.bfloat16)
    c["schn"] = (-np.sin(phc) / np.sqrt(128)).astype(ml_dtypes.bfloat16)
    c["ltri"] = (t[:, None] < t[None, :]).astype(np.float32)
    ee = np.arange(NE)
    c["le_mask"] = np.tile((ee[None, :] <= ee[:, None]).astype(np.float32).reshape(1, NE * NE), (128, 1))
    c["ones_b"] = np.ones((128, 128), np.float32).astype(ml_dtypes.bfloat16)
    c["iota_e"] = np.tile(np.arange(NE, dtype=np.float32)[None, :], (128, 1))
    c["iota_p"] = np.arange(128, dtype=np.float32)[:, None].copy()
    c["blk_thr"] = np.tile((np.arange(NBLK, dtype=np.float32) * BLK)[None, :], (128, 1))
    return c


CONST_DT = {"ident_f": F32, "ident_b": BF16, "tri": BF16, "ones_f": F32, "ctok": BF16, "stok": BF16,
            "cch": BF16, "schn": BF16, "ltri": F32, "le_mask": F32, "ones_b": BF16, "iota_e": F32, "iota_p": F32, "blk_thr": F32}


def build(debug=False):
    nc = bass.Bass("TRN2", target_bir_lowering=False)
    consts = _consts()

    def din(name, shape, dt=F32):
        return nc.dram_tensor(name, list(shape), dt, kind="ExternalInput").ap()

    def dscr(name, shape, dt):
        return nc.dram_tensor(name, list(shape), dt, kind="Internal").ap()

    x = din("x", [NB, L, D])
    ctx = din("ctx", [NB, LC, D])
    c5 = din("c5", [128, 8, 5])
    w_ada = din("w_ada", [D, 6 * D])
    b_adaT = din("b_adaT", [128, 48])
    b_ada = din("b_ada", [1, 6 * D])
    w_in = din("w_in", [D, 5120])
    lb_raw = din("lb_raw", [2, 2, 512])
    normgT = din("normgT", [128, 4])
    w_fo = din("w_four_out", [512, D])
    w_ho = din("w_hg_out", [512, D])
    w_o = din("w_o", [D, D])
    ln1_g = din("ln1_g", [1, D])
    ln1_b = din("ln1_b", [1, D])
    w_router = din("w_router", [D, NE])
    b_router = din("b_router", [1, NE])
    w_gu = din("w_gate_up", [NE, D, 2 * D])
    bguT = din("b_guT", [NE * 128, 16])
    w_dn = din("w_down", [NE, D, D])
    b_dn = din("b_down", [NE, D])
    ln2_g = din("ln2_g", [1, D])
    ln2_b = din("ln2_b", [1, D])
    cd = {k: din("k_" + k, v.shape, CONST_DT[k]) for k, v in consts.items()}
    out = nc.dram_tensor("out", [NB, L, D], F32, kind="ExternalOutput").ap()

    winb = dscr("winb", [D, 5120], BF16)
    wfob = dscr("wfob", [512, D], BF16)
    whob = dscr("whob", [512, D], BF16)
    wob = dscr("wob", [D, D], BF16)
    wgub = dscr("wgub", [NE * 128, 8 * 2 * D], BF16)
    wdnb = dscr("wdnb", [NE * 128, 8 * D], BF16)
    modrows = dscr("modrows", [5, 6 * D], F32)
    x1s = dscr("x1s", [NTOK, D], F32)
    u2b = dscr("u2b", [NTOK, D], BF16)
    xs = dscr("xs", [NROWS, D], BF16)
    ys = dscr("ys", [NROWS, D], F32)
    dbg = {}
    if debug:
        dbg["x1"] = nc.dram_tensor("dbg_x1", [NTOK, D], F32, kind="ExternalOutput").ap()
        dbg["yh"] = nc.dram_tensor("dbg_yh", [128, 4, L], F32, kind="ExternalOutput").ap()
        dbg["yf"] = nc.dram_tensor("dbg_yf", [128, 4, L], F32, kind="ExternalOutput").ap()
        dbg["mod"] = nc.dram_tensor("dbg_mod", [128, 48, 5], F32, kind="ExternalOutput").ap()

    with ExitStack() as es:
        p = Prog(nc, es)

        uniq = [0]

        def sb(scope, name, shape, dt=F32):
            uniq[0] += 1
            return scope.enter_context(nc.sbuf_tensor(f"{name}_{uniq[0]}", list(shape), dt))

        ps = [es.enter_context(nc.psum_tensor(f"ps{i}", [128, 512], F32)) for i in range(8)]
        psr = [R(f"ps{i}") for i in range(8)]
        bank_i = [0]

        bank_skip = [None]

        def bank():
            i = bank_i[0]
            if i == bank_skip[0]:
                i = (i + 1) % 8
            bank_i[0] = (i + 1) % 8
            return ps[i], psr[i]

        def mm(groups, reads, writes):
            def fn(e):
                inst = None
                for out_ap, pairs in groups:
                    n = len(pairs)
                    for i, (l, r_) in enumerate(pairs):
                        inst = e.matmul(out_ap, l, r_, start=(i == 0), stop=(i == n - 1))
                return inst
            p.op("pe", fn, reads=reads, writes=writes)

        def tr(out_ap, in_ap, ident, reads, writes):
            p.op("pe", lambda e: e.transpose(out_ap, in_ap, ident), reads=reads, writes=writes)

        def act(out_ap, in_ap, func, reads, writes, bias=None, scale=None):
            kw = {}
            if bias is not None:
                kw["bias"] = bias
            if scale is not None:
                kw["scale"] = scale
            p.op("act", lambda e: e.activation(out_ap, in_ap, func, **kw), reads=reads, writes=writes)

        def tt(eng, out_ap, a, b, op, reads, writes):
            p.op(eng, lambda e: e.tensor_tensor(out_ap, a, b, op), reads=reads, writes=writes)

        def ts(eng, out_ap, a, s1, s2, op0, op1, reads, writes):
            if op1 is None:
                p.op(eng, lambda e: e.tensor_scalar(out_ap, a, s1, None, op0), reads=reads, writes=writes)
            else:
                p.op(eng, lambda e: e.tensor_scalar(out_ap, a, s1, s2, op0, op1), reads=reads, writes=writes)

        def stt(out_ap, a, s, b, op0, op1, reads, writes):
            p.op("dve", lambda e: e.scalar_tensor_tensor(out_ap, a, s, b, op0, op1), reads=reads, writes=writes)

        def cp(eng, out_ap, in_ap, reads, writes):
            if eng == "act":
                p.op("act", lambda e: e.copy(out_ap, in_ap), reads=reads, writes=writes)
            else:
                p.op(eng, lambda e: e.tensor_copy(out_ap, in_ap), reads=reads, writes=writes)

        def dma(eng, out_ap, in_ap, reads, writes, ch, wacc=(), **kw):
            p.op(eng, lambda e: e.dma_start(out=out_ap, in_=in_ap, **kw), reads=reads, writes=writes, ch=ch, wacc=wacc)

        def dbgout(name, ap, shape, dt, reads):
            if not debug:
                return
            t = nc.dram_tensor("dbg_" + name, list(shape), dt, kind="ExternalOutput").ap()
            dma("pool", t, ap, reads, [R()], ch="dbg")

        k = {}
        kr = {}
        for name, arr in consts.items():
            if name in ("ctok", "stok", "le_mask"):
                continue
            k[name] = sb(es, "c_" + name, arr.shape, CONST_DT[name])
            kr[name] = R(name)
            dma("sp", k[name][:], cd[name], [], [kr[name]], ch="const")
        KR = list(kr.values())

        r_winb, r_wfob, r_whob, r_wob, r_wgub, r_wdnb = [R() for _ in range(6)]
        for j in range(10):
            dma("pool", winb[:, j * 512:(j + 1) * 512], w_in[:, j * 512:(j + 1) * 512], [], [], ch="cast_in", wacc=[r_winb])
        dma("pool", wfob, w_fo, [], [r_wfob], ch="cast_fo")
        dma("pool", whob, w_ho, [], [r_whob], ch="cast_ho")
        dma("pool", wob, w_o, [], [r_wob], ch="cast_o")
        zt = sb(es, "zt", [128, D], BF16)
        r_zt = R()
        r_zf = R()
        p.op("dve", lambda e: e.memset(zt[:], 0.0), writes=[r_zt])
        ZR = 4096
        for c_ in range(NROWS // ZR):
            dma("pool", xs[c_ * ZR:(c_ + 1) * ZR, :].rearrange("(n p) d -> p n d", p=128),
                zt[:].unsqueeze(1).to_broadcast([128, ZR // 128, D]), [r_zt], [], ch="zf", wacc=[r_zf])
        modT = sb(es, "modT", [128, 48, 5])
        r_modT = R()
        r_modrows = R()
        lbt = sb(es, "lbt", [128, 2, 512])
        r_lbt = R()
        normg = sb(es, "normg", [128, 4])
        r_normg = R()
        dma("sp", normg[:], normgT, [], [r_normg], ch="const")
        wr = sb(es, "wr", [128, 8, NE])
        r_wr = R()
        dma("sp", wr[:], w_router.rearrange("(kc p) e -> p kc e", p=128), [], [r_wr], ch="const")
        brt = sb(es, "brt", [128, NE])
        r_brt = R()
        dma("sp", brt[:], b_router.partition_broadcast(128), [], [r_brt], ch="const")
        for r_ in KR + [r_normg, r_wr, r_brt]:
            r_.w = {"d_const": p.cnt["d_const"]}
        epst = sb(es, "epst", [128, 3])
        r_eps = R()
        p.op("dve", lambda e: e.memset(epst[:, 0:1], RMS_EPS), writes=[r_eps])
        p.op("dve", lambda e: e.memset(epst[:, 1:2], LN_EPS), writes=[r_eps])
        p.op("dve", lambda e: e.memset(epst[:, 2:3], 1.0), writes=[r_eps])
        r_x1s, r_u2b, r_xs, r_ys = R(), R(), R(), R()
        Ltab = sb(es, "Ltab", [128, NTT, NE])
        Vtab = sb(es, "Vtab", [128, NTT, 8])
        r_tab = [R() for _ in range(NTT)]

        with ExitStack() as s0:
            cact = sb(s0, "cact", [128, 8, 5])
            r_cact = R()
            dma("sp", cact[:], c5, [], [r_cact], ch="p0a")
            badaT = sb(s0, "badaT", [128, 48])
            r_badaT = R()
            dma("sp", badaT[:], b_adaT, [], [r_badaT], ch="p0a")
            bada5 = sb(s0, "bada5", [5, 6 * D])
            r_bada5 = R()
            dma("sp", bada5[:], b_ada.partition_broadcast(5), [], [r_bada5], ch="p0a")
            lraw = sb(s0, "lraw", [128, 2, 2, 512])
            r_lraw = R()
            dma("sp", lraw[:], lb_raw.partition_broadcast(128), [], [r_lraw], ch="p0a")
            for r_ in (r_cact, r_badaT, r_bada5, r_lraw):
                r_.w = {"d_p0a": p.cnt["d_p0a"]}
            act(cact[:], cact[:], AF.Silu, [r_cact], [r_cact])
            rows = sb(s0, "rows", [5, 6 * D])
            r_rows = R()
            wa = [sb(s0, f"wa{i}", [128, 8, 512]) for i in range(2)]
            r_wa = [R(), R()]
            for g in range(12):
                i = g % 2
                dma("sp", wa[i][:], w_ada[:, g * 512:(g + 1) * 512].rearrange("(kc p) n -> p kc n", p=128),
                    [], [r_wa[i]], ch=f"wa{i}")
                pr, r_pr = bank()
                mm([(pr[0:5, :], [(cact[:, kc, :], wa[i][:, kc, :]) for kc in range(8)])], [r_wa[i], r_cact], [r_pr])
                tt("dve", rows[:, g * 512:(g + 1) * 512], pr[0:5, :], bada5[:, g * 512:(g + 1) * 512], ALU.add,
                   [r_pr, r_bada5], [r_rows])
            for j0 in (8, 32):
                ts("dve", rows[:, j0 * 128:(j0 + 8) * 128], rows[:, j0 * 128:(j0 + 8) * 128], 1.0, None, ALU.add, None,
                   [r_rows], [r_rows])
            pm, r_pm = bank()
            for j in range(48):
                tr(pm[:, j * 5:(j + 1) * 5], rows[0:5, j * 128:(j + 1) * 128], k["ident_f"][0:5, 0:5], [r_rows, kr["ident_f"]], [r_pm])
            cp("dve", modT[:], pm[:, 0:240].rearrange("p (j b) -> p j b", b=5), [r_pm], [r_modT])
            dma("sp", modrows, rows[:], [r_rows], [r_modrows], ch="p0s")
            if debug:
                dma("pool", dbg["mod"], modT[:], [r_modT], [R()], ch="dbg")
            ldiff = sb(s0, "ldiff", [128, 2, 512])
            r_ldiff = R()
            tt("dve", ldiff[:], lraw[:, 0, :, :], lraw[:, 1, :, :], ALU.subtract, [r_lraw], [r_ldiff])
            for dr in range(2):
                act(lbt[:, dr, :], ldiff[:, dr, :], AF.Sigmoid, [r_ldiff], [r_lbt], scale=-1.0)
                act(lbt[:, dr, :], lbt[:, dr, :], AF.Ln, [r_lbt], [r_lbt])

        p.fence()
        pending_casts = []
        for e_ in range(NE):
            pending_casts.append((wgub[e_ * 128:(e_ + 1) * 128, :].rearrange("p (kc f) -> p kc f", kc=8),
                                  w_gu[e_].rearrange("(kc p) f -> p kc f", p=128), r_wgub))
            pending_casts.append((wdnb[e_ * 128:(e_ + 1) * 128, :].rearrange("p (kc f) -> p kc f", kc=8),
                                  w_dn[e_].rearrange("(kc p) f -> p kc f", p=128), r_wdnb))

        def issue_casts(n):
            for _ in range(min(n, len(pending_casts))):
                o_, i_, r_ = pending_casts.pop(0)
                dma("pool", o_, i_, [], [], ch="cast", wacc=[r_])

        with ExitStack() as sm:
            uT = sb(sm, "uT", [128, 8, L], BF16)
            r_uT = R()
            ucT = sb(sm, "ucT", [128, 8, LC], BF16)
            r_ucT = R()
            wg = [sb(sm, f"wg{i}", [128, 8, 512], BF16) for i in range(2)]
            r_wg = [R(), R()]
            wg_i = [0]
            yfT = sb(sm, "yfT", [128, 4, L], BF16)
            r_yfT = R()
            yhT = sb(sm, "yhT", [128, 4, L], BF16)
            r_yhT = R()

            def load_wg(c0, ncols=512):
                i = wg_i[0]
                wg_i[0] = 1 - i
                dma("sp", wg[i][:, :, 0:ncols], winb[:, c0:c0 + ncols].rearrange("(kc p) n -> p kc n", p=128),
                    [r_winb], [r_wg[i]], ch=f"wg{i}")
                return wg[i], r_wg[i]

            for b in range(NB):
                p.fence()
                with ExitStack() as s1:
                    xt = [sb(s1, f"xt{i}", [128, 4, D]) for i in range(2)]
                    r_xt = [R(), R()]

                    def make_uT(src, ntiles, dst, r_dst, col):
                        ngr = (ntiles + 3) // 4
                        for g in range(ngr):
                            i = g % 2
                            nt = min(4, ntiles - g * 4)
                            dma("sp", xt[i][:, 0:nt, :],
                                src[g * 512:g * 512 + nt * 128, :].rearrange("(a p) d -> p a d", p=128),
                                [], [r_xt[i]], ch=f"xt{i}")
                            for kc in range(8):
                                pb, r_pb = bank()
                                for a in range(nt):
                                    tr(pb[:, a * 128:(a + 1) * 128], xt[i][:, a, kc * 128:(kc + 1) * 128], k["ident_f"][:],
                                       [r_xt[i], kr["ident_f"]], [r_pb])
                                act(dst[:, kc, g * 512:g * 512 + nt * 128], pb[:, 0:nt * 128], AF.Identity,
                                    [r_pb, r_modT], [r_dst], bias=modT[:, kc, col:col + 1], scale=modT[:, 8 + kc, col:col + 1])
                    make_uT(x[b], NT, uT, r_uT, b)
                    make_uT(ctx[b], 2, ucT, r_ucT, 4)
                    if b == 0:
                        dbgout("uT", uT[:], [128, 8, L], BF16, [r_uT])

                p.fence()
                with ExitStack() as s3:
                    qT = sb(s3, "qT", [128, 4, L], BF16)
                    r_qT = R()
                    vtm = sb(s3, "vtm", [128, NT, 512], BF16)
                    r_vtm = R()
                    vctm = sb(s3, "vctm", [128, 2, 512], BF16)
                    r_vctm = R()
                    oacc = sb(s3, "oacc", [128, 4, L], BF16)
                    r_oacc = [R() for _ in range(NT)]
                    LF32 = sb(s3, "LF32", [128, 512])
                    LK32 = sb(s3, "LK32", [128, 512])
                    r_LF32, r_LK32 = R(), R()
                    LFh = [sb(s3, f"LFh{d_}", [128, 512], BF16) for d_ in range(2)]
                    LFl = [sb(s3, f"LFl{d_}", [128, 512], BF16) for d_ in range(2)]
                    kb = [sb(s3, f"kb{d_}", [128, 512], BF16) for d_ in range(2)]
                    eqn = [sb(s3, f"eqn{d_}", [128, 512]) for d_ in range(2)]
                    er = sb(s3, "er", [128, 512], BF16)
                    r_eqn = [R(), R()]
                    r_er = R()
                    r_LF = [R(), R()]
                    r_LK = [R(), R()]
                    sg = sb(s3, "sg", [128, 512])
                    sgn = sb(s3, "sgn", [128, 512])
                    r_sg, r_sgn = R(), R()
                    eq = [sb(s3, f"eq{d_}", [128, 512]) for d_ in range(2)]
                    r_eq = [R(), R()]
                    qdec = [sb(s3, f"qdec{d_}", [128, 4, 128], BF16) for d_ in range(2)]
                    kinv = [sb(s3, f"kinv{d_}", [128, 4, 128], BF16) for d_ in range(2)]
                    kend = [sb(s3, f"kend{d_}", [128, 512], BF16) for d_ in range(2)]
                    scm = [sb(s3, f"scm{d_}", [128, 4, 128], BF16) for d_ in range(2)]
                    r_qdec, r_kinv, r_kend, r_scm = [[R(), R()] for _ in range(4)]
                    S = [sb(s3, f"S{d_}", [128, 4, 128]) for d_ in range(2)]
                    Sb = [sb(s3, f"Sb{d_}", [128, 4, 4, 128], BF16) for d_ in range(2)]
                    r_S = [R(), R()]
                    r_Sb = [[R() for _ in range(4)] for _ in range(2)]

                    wq, r_wq = load_wg(512)
                    for tq in range(4):
                        for h in range(4):
                            pb, r_pb = bank()
                            mm([(pb[:], [(wq[:, kc, h * 128:(h + 1) * 128], uT[:, kc, tq * 512:(tq + 1) * 512]) for kc in range(8)])],
                               [r_wq, r_uT], [r_pb])
                            act(qT[:, h, tq * 512:(tq + 1) * 512], pb[:], AF.Silu, [r_pb], [r_qT])
                    wv, r_wv = load_wg(2048)
                    for ti in range(NT):
                        pb, r_pb = bank()
                        mm([(pb[:], [(uT[:, kc, ti * 128:(ti + 1) * 128], wv[:, kc, :]) for kc in range(8)])], [r_wv, r_uT], [r_pb])
                        cp("act", vtm[:, ti, :], pb[:], [r_pb], [r_vtm])
                    for ti in range(2):
                        pb, r_pb = bank()
                        mm([(pb[:], [(ucT[:, kc, ti * 128:(ti + 1) * 128], wv[:, kc, :]) for kc in range(8)])], [r_wv, r_ucT], [r_pb])
                        cp("act", vctm[:, ti, :], pb[:], [r_pb], [r_vctm])
                    wz = [None, None]
                    r_wz = [None, None]
                    wz[0], r_wz[0] = load_wg(1024)
                    wz[1], r_wz[1] = load_wg(1536)

                    for dr in range(2):
                        p.op("dve", lambda e, dr=dr, S=S: e.memset(S[dr][:], 0.0), writes=[r_S[dr]])
                        p.op("dve", lambda e, dr=dr, Sb=Sb: e.memset(Sb[dr][:], 0.0), writes=r_Sb[dr])

                    def gla_step(dr, step, ti, srcT, r_src, vt, r_vt, latent, first_write):
                        tri = k["tri"]
                        issue_casts(1)
                        pz, r_pz = bank()
                        mm([(pz[:], [(srcT[:, kc, ti * 128:(ti + 1) * 128], wz[dr][:, kc, :]) for kc in range(8)])],
                           [r_wz[dr], r_src], [r_pz])
                        act(sg[:], pz[:], AF.Exp, [r_pz], [r_sg])
                        act(sgn[:], sg[:], AF.Ln, [r_sg, r_eps], [r_sgn], bias=epst[:, 2:3], scale=1.0)
                        tt("dve", LK32[:], lbt[:, dr, :], sgn[:], ALU.subtract, [r_sgn, r_lbt], [r_LK32])
                        act(sg[:], LK32[:], AF.Exp, [r_LK32], [r_sg])
                        act(LF32[:], sg[:], AF.Ln, [r_sg, r_eps], [r_LF32], bias=epst[:, 2:3], scale=-1.0)
                        cp("act", LFh[dr][:], LF32[:], [r_LF32], [r_LF[dr]])
                        tt("dve", LFl[dr][:], LF32[:], LFh[dr][:], ALU.subtract, [r_LF32, r_LF[dr]], [r_LF[dr]])
                        cp("dve", kb[dr][:], sg[:], [r_sg], [r_LK[dr]])
                        pq, r_pq = bank()
                        mm([(pq[:, h * 128:(h + 1) * 128], [(LFh[dr][:, h * 128:(h + 1) * 128], tri[:, dr, 0, :]),
                                                           (LFl[dr][:, h * 128:(h + 1) * 128], tri[:, dr, 0, :])]) for h in range(4)],
                           [r_LF[dr], kr["tri"]], [r_pq])
                        act(eq[dr][:], pq[:], AF.Exp, [r_pq], [r_eq[dr]])
                        if latent:
                            act(eqn[dr][:], pq[:], AF.Exp, [r_pq], [r_eqn[dr]], scale=-1.0)
                        pe_, r_pe = bank()
                        mm([(pe_[:], [(tri[:, dr, 1, :], LFh[dr][:]), (tri[:, dr, 1, :], LFl[dr][:])])],
                           [r_LF[dr], kr["tri"]], [r_pe])
                        act(er[:], pe_[:], AF.Exp, [r_pe], [r_er])
                        tt("dve", kend[dr][:], er[:], kb[dr][:], ALU.mult, [r_er, r_LK[dr]], [r_kend[dr]])
                        if latent:
                            pk, r_pk = bank()
                            mm([(pk[:, h * 128:(h + 1) * 128], [(kb[dr][:, h * 128:(h + 1) * 128], k["ident_b"][:])]) for h in range(4)],
                               [r_LK[dr], kr["ident_b"]], [r_pk])
                            tt("dve", kinv[dr][:].rearrange("p h t -> p (h t)"), pk[:], eqn[dr][:], ALU.mult, [r_pk, r_eqn[dr]], [r_kinv[dr]])
                            tt("dve", qdec[dr][:], eq[dr][:].rearrange("p (h t) -> p h t", h=4), qT[:, :, ti * 128:(ti + 1) * 128], ALU.mult,
                               [r_eq[dr], r_qT], [r_qdec[dr]])
                            psc, r_psc = bank()
                            mm([(psc[:, h * 128:(h + 1) * 128], [(kinv[dr][:, h, :], qdec[dr][:, h, :])]) for h in range(4)],
                               [r_kinv[dr], r_qdec[dr]], [r_psc])
                            tt("dve", scm[dr][:], psc[:].rearrange("p (h t) -> p h t", h=4),
                               tri[:, dr, 0, :].unsqueeze(1).to_broadcast([128, 4, 128]), ALU.mult, [r_psc, kr["tri"]], [r_scm[dr]])
                        chunks = (0, 1) if dr == 0 else (1, 0)
                        sl = (step % 2) * 2
                        nsl = ((step + 1) % 2) * 2
                        for ci, ch_ in enumerate(chunks):
                            c0 = ch_ * 64
                            gcol = c0 + 63 if dr == 0 else c0
                            pkv, r_pkv = bank()
                            mm([(pkv[:, h * 128:(h + 1) * 128], [(kend[dr][c0:c0 + 64, h * 128:(h + 1) * 128], vt[c0:c0 + 64, ti, h * 128:(h + 1) * 128])])
                                for h in range(4)], [r_kend[dr], r_vt], [r_pkv])
                            for h in range(4):
                                stt(S[dr][:, h, :], S[dr][:, h, :], eq[dr][:, h * 128 + gcol:h * 128 + gcol + 1], pkv[:, h * 128:(h + 1) * 128],
                                    ALU.mult, ALU.add, [r_S[dr], r_eq[dr], r_pkv], [r_S[dr]])
                            dst = sl + 1 if ci == 0 else nsl
                            cp("act", Sb[dr][:, dst, :, :], S[dr][:], [r_S[dr]], [r_Sb[dr][dst]])
                        if latent:
                            po, r_po = bank()

                            def fn(e, dr=dr, ti=ti, chunks=chunks, sl=sl, po=po, vt=vt, scm=scm, Sb=Sb, qdec=qdec):
                                inst = None
                                for h in range(4):
                                    inst = e.matmul(po[:, h * 128:(h + 1) * 128], vt[:, ti, h * 128:(h + 1) * 128], scm[dr][:, h, :],
                                                    start=True, stop=False)
                                    for ci, ch_ in enumerate(chunks):
                                        c0 = ch_ * 64
                                        inst = e.matmul(po[:, h * 128 + c0:h * 128 + c0 + 64], Sb[dr][:, sl + ci, h, :],
                                                        qdec[dr][:, h, c0:c0 + 64], start=False, stop=(ci == 1))
                                return inst
                            p.op("pe", fn, reads=[r_vt, r_scm[dr], r_qdec[dr], r_Sb[dr][sl], r_Sb[dr][sl + 1]], writes=[r_po])
                            ov = oacc[:, :, ti * 128:(ti + 1) * 128]
                            pov = po[:].rearrange("p (h t) -> p h t", h=4)
                            if first_write:
                                cp("act", ov, pov, [r_po], [r_oacc[ti]])
                            else:
                                tt("dve", ov, pov, ov, ALU.add, [r_po, r_oacc[ti]], [r_oacc[ti]])

                    for i in range(2):
                        gla_step(0, i, i, ucT, r_ucT, vctm, r_vctm, False, False)
                        gla_step(1, i, 1 - i, ucT, r_ucT, vctm, r_vctm, False, False)
                    for i in range(NT):
                        gla_step(0, i + 2, i, uT, r_uT, vtm, r_vtm, True, i < NT // 2)
                        gla_step(1, i + 2, NT - 1 - i, uT, r_uT, vtm, r_vtm, True, i < NT // 2)

                    if b == 0:
                        dbgout("qT", qT[:], [128, 4, L], BF16, [r_qT])
                        dbgout("oacc", oacc[:], [128, 4, L], BF16, r_oacc)
                        dbgout("Sf", S[0][:], [128, 4, 128], F32, [r_S[0]])
                    wgg, r_wgg = load_wg(2560)
                    sq = sb(s3, "sq", [128, 512])
                    r_sq = R()
                    rstd = sb(s3, "rstd", [128, 512])
                    r_rstd = R()
                    sgate = sb(s3, "sgate", [128, 512])
                    r_sgate = R()
                    r_ALL_o = r_oacc
                    for tq in range(4):
                        for h in range(4):
                            osl = oacc[:, h, tq * 512:(tq + 1) * 512]
                            act(sq[:], osl, AF.Square, r_ALL_o[tq * 4:(tq + 1) * 4], [r_sq])
                            pb, r_pb = bank()
                            mm([(pb[:], [(k["ones_f"][:], sq[:])])], [r_sq, kr["ones_f"]], [r_pb])
                            act(rstd[:], pb[:], AF.Sqrt, [r_pb, r_eps], [r_rstd], bias=epst[:, 0:1], scale=1.0 / 128.0)
                            p.op("dve", lambda e, rstd=rstd: e.reciprocal(rstd[:], rstd[:]), reads=[r_rstd], writes=[r_rstd])
                            pg_, r_pg = bank()
                            mm([(pg_[:], [(wgg[:, kc, h * 128:(h + 1) * 128], uT[:, kc, tq * 512:(tq + 1) * 512]) for kc in range(8)])],
                               [r_wgg, r_uT], [r_pg])
                            act(sgate[:], pg_[:], AF.Silu, [r_pg], [r_sgate])
                            tt("dve", rstd[:], rstd[:], osl, ALU.mult, [r_rstd] + r_ALL_o[tq * 4:(tq + 1) * 4], [r_rstd])
                            stt(yhT[:, h, tq * 512:(tq + 1) * 512], rstd[:], normg[:, h:h + 1], sgate[:], ALU.mult, ALU.mult,
                                [r_rstd, r_normg, r_sgate], [r_yhT])

                if b == 0:
                    dbgout("yhT", yhT[:], [128, 4, L], BF16, [r_yhT])
                p.fence()
                with ExitStack() as s2:
                    pftm = sb(s2, "pftm", [128, NT, 512], BF16)
                    r_pftm = R()
                    dft = [sb(s2, f"dft{i}", [128, NT, 512], BF16) for i in range(2)]
                    r_dft = [R(), R()]
                    AB = sb(s2, "AB", [128, 2, 4, 512], BF16)
                    r_AB = [R(), R()]
                    wf, r_wf = load_wg(0)
                    for ti in range(NT):
                        pb, r_pb = bank()
                        mm([(pb[:], [(uT[:, kc, ti * 128:(ti + 1) * 128], wf[:, kc, :]) for kc in range(8)])], [r_wf, r_uT], [r_pb])
                        cp("act" if ti % 2 else "dve", pftm[:, ti, :], pb[:], [r_pb], [r_pftm])
                    srcs = (cd["ctok"], cd["stok"])
                    for tq in range(4):
                        for cs in range(2):
                            dma("sp", dft[cs][:], srcs[cs][:, tq * 512:(tq + 1) * 512].rearrange("(ti p) t -> p ti t", p=128),
                                [], [r_dft[cs]], ch=f"dft{cs}")
                            for g in range(4):
                                pb, r_pb = bank()
                                mm([(pb[:], [(pftm[:, ti, g * 128:(g + 1) * 128], dft[cs][:, ti, :]) for ti in range(NT)])],
                                   [r_pftm, r_dft[cs]], [r_pb])
                                cp("act" if g % 2 else "dve", AB[:, cs, g, :], pb[:], [r_pb], [r_AB[cs]])
                        for g in range(4):
                            pb, r_pb = bank()
                            mm([(pb[:], [(k["cch"][:], AB[:, 0, g, :]), (k["schn"][:], AB[:, 1, g, :])])],
                               [r_AB[0], r_AB[1], kr["cch"], kr["schn"]], [r_pb])
                            cp("act" if g % 2 else "dve", yfT[:, g, tq * 512:(tq + 1) * 512], pb[:], [r_pb], [r_yfT])

                if b == 0:
                    dbgout("yfT", yfT[:], [128, 4, L], BF16, [r_yfT])
                p.fence()
                with ExitStack() as s4:
                    wfo_t = sb(s4, "wfo_t", [128, 4, D], BF16)
                    who_t = sb(s4, "who_t", [128, 4, D], BF16)
                    wo_t = sb(s4, "wo_t", [128, 8, D], BF16)
                    r_wfo_t, r_who_t, r_wo_t = R(), R(), R()
                    dma("sp", wfo_t[:], wfob.rearrange("(kc p) n -> p kc n", p=128), [r_wfob], [r_wfo_t], ch="wm0")
                    dma("sp", who_t[:], whob.rearrange("(kc p) n -> p kc n", p=128), [r_whob], [r_who_t], ch="wm1")
                    dma("sp", wo_t[:], wob.rearrange("(kc p) n -> p kc n", p=128), [r_wob], [r_wo_t], ch="wm2")
                    bc = sb(s4, "bc", [128, 5, D])
                    r_bc = R()
                    for i, j0 in enumerate((16, 32, 24)):
                        dma("sp", bc[:, i, :], modrows[b:b + 1, j0 * 128:(j0 + 8) * 128].partition_broadcast(128), [r_modrows], [r_bc], ch="bc")
                    dma("sp", bc[:, 3, :], ln1_g.partition_broadcast(128), [], [r_bc], ch="bc")
                    dma("sp", bc[:, 4, :], ln1_b.partition_broadcast(128), [], [r_bc], ch="bc")
                    mTs = [sb(s4, f"mT{i}", [128, 8, 512], BF16) for i in range(2)]
                    r_mTs = [R(), R()]
                    sgf = sb(s4, "sgf", [128, 512])
                    sgh = sb(s4, "sgh", [128, 512])
                    r_sgf, r_sgh = R(), R()
                    xr = [sb(s4, f"xr{i}", [128, D]) for i in range(2)]
                    r_xr = [R(), R()]
                    tbuf = sb(s4, "tbuf", [128, D])
                    r_tbuf = R()
                    x1t = sb(s4, "x1t", [128, D])
                    r_x1t = R()
                    u2t = sb(s4, "u2t", [128, D])
                    r_u2t = R()
                    u2bt = sb(s4, "u2bt", [128, D], BF16)
                    r_u2bt = R()
                    u2T = sb(s4, "u2T", [128, 8, 128])
                    r_u2T = R()
                    stats = sb(s4, "stats", [128, 2, 6])
                    mv = sb(s4, "mv", [128, 2])
                    nmr = sb(s4, "nmr", [128, 2])
                    r_stats, r_mv, r_nmr = R(), R(), R()

                    def layer_norm(src, r_src, dst, r_dst, gidx, bidx, bct, r_bct):
                        for hf in range(2):
                            p.op("dve", lambda e, hf=hf, stats=stats, src=src: e.bn_stats(stats[:, hf, :], src[:, hf * 512:(hf + 1) * 512]), reads=[r_src], writes=[r_stats])
                        p.op("dve", lambda e, mv=mv, stats=stats: e.bn_aggr(mv[:], stats[:].rearrange("p a s -> p (a s)")), reads=[r_stats], writes=[r_mv])
                        act(nmr[:, 0:1], mv[:, 1:2], AF.Sqrt, [r_mv, r_eps], [r_nmr], bias=epst[:, 1:2], scale=1.0)
                        p.op("dve", lambda e, nmr=nmr: e.reciprocal(nmr[:, 0:1], nmr[:, 0:1]), reads=[r_nmr], writes=[r_nmr])
                        stt(dst[:], src[:], mv[:, 0:1], bct[:, gidx, :], ALU.subtract, ALU.mult, [r_src, r_mv, r_bct], [r_dst])
                        stt(dst[:], dst[:], nmr[:, 0:1], bct[:, bidx, :], ALU.mult, ALU.add, [r_dst, r_nmr, r_bct], [r_dst])

                    gslot_r = [R() for _ in range(8)]
                    gseq = [(0, j_) for j_ in range(8)]
                    for tq_ in range(3):
                        gseq += [(tq_ + 1, j_) for j_ in range(8)]
                    gidx = {key: n_ for n_, key in enumerate(gseq)}
                    gate_w = {}

                    def load_gate(key):
                        tq_, j_ = key
                        n_ = gidx[key]
                        aps, rs = [], []
                        for br in range(2):
                            sl_ = (2 * n_ + br) % 8
                            ap_ = wg[sl_ // 4][:, :, (sl_ % 4) * 128:(sl_ % 4 + 1) * 128]
                            c0_ = 3072 + br * 1024 + j_ * 128
                            dma("sp", ap_, winb[:, c0_:c0_ + 128].rearrange("(kc p) n -> p kc n", p=128),
                                [r_winb], [gslot_r[sl_]], ch=f"gs{sl_}")
                            aps.append(ap_)
                            rs.append(gslot_r[sl_])
                        gate_w[key] = (aps, rs)

                    for n_ in range(3):
                        load_gate(gseq[n_])

                    def gates_j(tq, j):
                        wgj, r_wgj = gate_w[(tq, j)]
                        nxt = gidx[(tq, j)] + 3
                        if nxt < len(gseq):
                            load_gate(gseq[nxt])
                        for br, (sgt, r_sgt, yT, r_yT, wpt, r_wpt) in enumerate(((sgf, r_sgf, yfT, r_yfT, wfo_t, r_wfo_t),
                                                                               (sgh, r_sgh, yhT, r_yhT, who_t, r_who_t))):
                            pb, r_pb = bank()
                            mm([(pb[:], [(wgj[br][:, kc, :], uT[:, kc, tq * 512:(tq + 1) * 512]) for kc in range(8)])],
                               [r_wgj[br], r_uT], [r_pb])
                            act(sgt[:], pb[:], AF.Sigmoid, [r_pb], [r_sgt])
                            pb2, r_pb2 = bank()
                            mm([(pb2[:], [(wpt[:, kc, j * 128:(j + 1) * 128], yT[:, kc, tq * 512:(tq + 1) * 512]) for kc in range(4)])],
                               [r_wpt, r_yT], [r_pb2])
                            tt("dve", sgt[:], sgt[:], pb2[:], ALU.mult, [r_sgt, r_pb2], [r_sgt])
                        tt("dve", mTs[tq % 2][:, j, :], sgf[:], sgh[:], ALU.add, [r_sgf, r_sgh], [r_mTs[tq % 2]])
                    def ln_tile(tq, a):
                        ti = tq * 4 + a
                        gt = b * NT + ti
                        i = ti % 2
                        dma("sp", xr[i][:], x[b, ti * 128:(ti + 1) * 128, :], [], [r_xr[i]], ch=f"xr{i}")
                        for hf in range(2):
                            pb, r_pb = bank()
                            mm([(pb[:], [(mTs[tq % 2][:, kc, a * 128:(a + 1) * 128], wo_t[:, kc, hf * 512:(hf + 1) * 512]) for kc in range(8)])],
                               [r_mTs[tq % 2], r_wo_t], [r_pb])
                            tt("dve", tbuf[:, hf * 512:(hf + 1) * 512], pb[:], bc[:, 0, hf * 512:(hf + 1) * 512], ALU.mult, [r_pb, r_bc], [r_tbuf])
                        stt(tbuf[:], xr[i][:], ALPHA, tbuf[:], ALU.mult, ALU.add, [r_xr[i], r_tbuf], [r_tbuf])
                        layer_norm(tbuf, r_tbuf, x1t, r_x1t, 3, 4, bc, r_bc)
                        dma("sp", x1s[gt * 128:(gt + 1) * 128, :], x1t[:], [r_x1t], [], ch="x1s", wacc=[r_x1s])
                        if debug:
                            dma("pool", dbg["x1"][gt * 128:(gt + 1) * 128, :], x1t[:], [r_x1t], [R()], ch="dbg")
                        tt("dve", u2t[:], x1t[:], bc[:, 1, :], ALU.mult, [r_x1t, r_bc], [r_u2t])
                        tt("dve", u2t[:], u2t[:], bc[:, 2, :], ALU.add, [r_u2t, r_bc], [r_u2t])
                        cp("act", u2bt[:], u2t[:], [r_u2t], [r_u2bt])
                        dma("sp", u2b[gt * 128:(gt + 1) * 128, :], u2bt[:], [r_u2bt], [], ch="u2b", wacc=[r_u2b])
                        for hf in range(2):
                            pb, r_pb = bank()
                            for kk in range(4):
                                kc = hf * 4 + kk
                                tr(pb[:, kk * 128:(kk + 1) * 128], u2t[:, kc * 128:(kc + 1) * 128], k["ident_f"][:], [r_u2t, kr["ident_f"]], [r_pb])
                            cp("act", u2T[:, hf * 4:(hf + 1) * 4, :].rearrange("p a t -> p (a t)"), pb[:], [r_pb], [r_u2T])
                        pb, r_pb = bank()
                        mm([(pb[:, 0:NE], [(u2T[:, kc, :], wr[:, kc, :]) for kc in range(8)])], [r_u2T, r_wr], [r_pb])
                        tt("dve", Ltab[:, gt, :], pb[:, 0:NE], brt[:], ALU.add, [r_pb, r_brt], [r_tab[gt]])
                        p.op("dve", lambda e, gt=gt: e.max(Vtab[:, gt, :], Ltab[:, gt, :]), reads=[r_tab[gt]], writes=[r_tab[gt]])

                    for j in range(8):
                        gates_j(0, j)
                    for tq in range(4):
                        for a in range(4):
                            if tq + 1 < 4:
                                gates_j(tq + 1, 2 * a)
                                gates_j(tq + 1, 2 * a + 1)
                            ln_tile(tq, a)
        issue_casts(len(pending_casts))
        p.fence()
        dbgout("Ltab", Ltab[:], [128, NTT, NE], F32, r_tab)
        dbgout("Vtab", Vtab[:], [128, NTT, 8], F32, r_tab)
        desti = sb(es, "desti", [128, 4, NTT], I32)
        r_desti = R()
        Wtab = sb(es, "Wtab", [128, NTT, 4])
        r_Wtab = R()
        idxw = sb(es, "idxw", [128, NBLK], I32)
        idxe = sb(es, "idxe", [128, NBLK], I32)
        r_idx = R()
        ALLTAB = r_tab

        with ExitStack() as sd:
            pos = sb(sd, "pos", [128, NTT, NE])
            r_pos = R()
            Mtab = sb(sd, "Mtab", [128, NTT, NE])
            r_M = R()
            tt("dve", Mtab[:], Ltab[:], Vtab[:, :, 3:4].to_broadcast([128, NTT, NE]), ALU.is_ge, r_tab, [r_M])
            k["le_mask"] = sb(sd, "c_le_mask", [128, NE * NE])
            kr["le_mask"] = R()
            dma("sp", k["le_mask"][:], cd["le_mask"], [], [kr["le_mask"]], ch="const")
            run = sb(sd, "run", [128, NE])
            r_run = R()
            p.op("dve", lambda e: e.memset(run[:], 0.0), writes=[r_run])
            for gt in range(NTT):
                pb, r_pb = bank()
                mm([(pb[:, 0:NE], [(k["ltri"][:], Mtab[:, gt, :])]), (pb[:, NE:2 * NE], [(k["ones_f"][:], Mtab[:, gt, :])])],
                   [r_M, kr["ltri"], kr["ones_f"]], [r_pb])
                tt("dve", pos[:, gt, :], pb[:, 0:NE], run[:], ALU.add, [r_pb, r_run], [r_pos])
                tt("dve", run[:], pb[:, NE:2 * NE], run[:], ALU.add, [r_pb, r_run], [r_run])
            big = sb(sd, "big", [128, NE * NBLK])
            r_big = R()
            nblk = sb(sd, "nblk", [128, NE])
            padded = sb(sd, "padded", [128, NE])
            pend = sb(sd, "pend", [128, NE])
            pstart = sb(sd, "pstart", [128, NE])
            r_sm = R()
            tt("dve", big[:].rearrange("p (e j) -> p e j", e=NE), run[:].unsqueeze(2).to_broadcast([128, NE, NBLK]),
               k["blk_thr"][:].unsqueeze(1).to_broadcast([128, NE, NBLK]), ALU.is_gt, [r_run, kr["blk_thr"]], [r_big])
            p.op("dve", lambda e: e.reduce_sum(nblk[:], big[:].rearrange("p (e j) -> p e j", e=NE), axis=AX.X), reads=[r_big], writes=[r_sm])
            ts("dve", padded[:], nblk[:], float(BLK), None, ALU.mult, None, [r_sm], [r_sm])
            tt("dve", big[:, 0:NE * NE].rearrange("p (e f) -> p e f", e=NE), padded[:].unsqueeze(1).to_broadcast([128, NE, NE]),
               k["le_mask"][:].rearrange("p (e f) -> p e f", e=NE), ALU.mult, [r_sm, kr["le_mask"], r_big], [r_big])
            p.op("dve", lambda e: e.reduce_sum(pend[:], big[:, 0:NE * NE].rearrange("p (e f) -> p e f", e=NE), axis=AX.X), reads=[r_big], writes=[r_sm])
            tt("dve", pstart[:], pend[:], padded[:], ALU.subtract, [r_sm], [r_sm])
            tt("dve", big[:].rearrange("p (j e) -> p j e", e=NE), pend[:].unsqueeze(1).to_broadcast([128, NBLK, NE]),
               k["blk_thr"][:].unsqueeze(2).to_broadcast([128, NBLK, NE]), ALU.is_le, [r_sm, kr["blk_thr"], r_big], [r_big])
            bef = sb(sd, "bef", [128, NBLK])
            r_bef = R()
            p.op("dve", lambda e: e.reduce_sum(bef[:], big[:].rearrange("p (j e) -> p j e", e=NE), axis=AX.X), reads=[r_big], writes=[r_bef])
            ts("dve", bef[:], bef[:], float(NE - 1), None, ALU.min, None, [r_bef], [r_bef])
            cp("dve", idxe[:], bef[:], [r_bef], [r_idx])
            ts("dve", bef[:], bef[:], 128.0, k["iota_p"][:, 0:1], ALU.mult, ALU.add, [r_bef, kr["iota_p"]], [r_bef])
            cp("dve", idxw[:], bef[:], [r_bef], [r_idx])
            tt("dve", pos[:], pos[:], pstart[:].unsqueeze(1).to_broadcast([128, NTT, NE]), ALU.add, [r_pos, r_sm], [r_pos])
            oh = sb(sd, "oh", [128, NTT, NE])
            r_oh = R()
            destf = sb(sd, "destf", [128, 4, NTT])
            r_destf = R()
            for kk in range(4):
                tt("dve", oh[:], Ltab[:], Vtab[:, :, kk:kk + 1].to_broadcast([128, NTT, NE]), ALU.is_equal, ALLTAB, [r_oh])
                tt("dve", oh[:], oh[:], pos[:], ALU.mult, [r_oh, r_pos], [r_oh])
                p.op("dve", lambda e, kk=kk: e.reduce_sum(destf[:, kk, :], oh[:], axis=AX.X), reads=[r_oh], writes=[r_destf])
            cp("dve", desti[:], destf[:], [r_destf], [r_desti])
            ew = sb(sd, "ew", [128, NTT, 4])
            r_ew = R()
            ssum = sb(sd, "ssum", [128, NTT])
            r_ssum = R()
            tt("dve", ew[:], Vtab[:, :, 0:4], Vtab[:, :, 0:1].to_broadcast([128, NTT, 4]), ALU.subtract, ALLTAB, [r_ew])
            act(ew[:], ew[:], AF.Exp, [r_ew], [r_ew])
            p.op("dve", lambda e: e.reduce_sum(ssum[:], ew[:], axis=AX.X), reads=[r_ew], writes=[r_ssum])
            p.op("dve", lambda e: e.reciprocal(ssum[:], ssum[:]), reads=[r_ssum], writes=[r_ssum])
            tt("dve", Wtab[:], ew[:], ssum[:].unsqueeze(2).to_broadcast([128, NTT, 4]), ALU.mult, [r_ew, r_ssum], [r_Wtab])
            ub = [sb(sd, f"ub{i}", [128, D], BF16) for i in range(2)]
            r_ub = [R(), R()]
            for gt in range(NTT):
                i = gt % 2
                dma("sp", ub[i][:], u2b[gt * 128:(gt + 1) * 128, :], [r_u2b], [r_ub[i]], ch=f"ub{i}")
                for kk in range(4):
                    p.op("pool", lambda e, i=i, kk=kk, gt=gt: e.indirect_dma_start(
                        out=xs, out_offset=bass.IndirectOffsetOnAxis(ap=desti[:, kk, gt:gt + 1], axis=0), in_=ub[i][:], in_offset=None),
                        reads=[r_ub[i], r_desti, r_zf], wacc=[r_xs], ch=f"sc{i}")

        dbgout("desti", desti[:], [128, 4, NTT], I32, [r_desti])
        dbgout("Wtab", Wtab[:], [128, NTT, 4], F32, [r_Wtab])
        dbgout("idxw", idxw[:], [128, NBLK], I32, [r_idx])
        p.fence()
        with ExitStack() as se:
            wgu_t = [sb(se, f"wgu_t{i}", [128, 8 * 2 * D], BF16) for i in range(2)]
            wdn_t = [sb(se, f"wdn_t{i}", [128, 8 * D], BF16) for i in range(2)]
            bgu_t = [sb(se, f"bgu_t{i}", [128, 16]) for i in range(2)]
            bdn_t = [sb(se, f"bdn_t{i}", [128, D]) for i in range(2)]
            r_wt = [R(), R()]
            xrows = [sb(se, f"xrows{i}", [128, 4, D], BF16) for i in range(2)]
            r_xrows = [R(), R()]
            xsT = [sb(se, f"xsT{i}", [128, 8, BLK], BF16) for i in range(2)]
            r_xsT = [R(), R()]
            actT = sb(se, "actT", [128, 8, BLK], BF16)
            r_actT = [R() for _ in range(8)]
            gc = [sb(se, f"gc{i}", [128, BLK]) for i in range(2)]
            sgm = [sb(se, f"sgm{i}", [128, BLK]) for i in range(2)]
            uu = [sb(se, f"uu{i}", [128, BLK]) for i in range(2)]
            r_gc, r_sgm, r_uu = [R(), R()], [R(), R()], [R(), R()]
            yst = [sb(se, f"yst{i}", [128, D]) for i in range(2)]
            r_yst = [R(), R()]

            def load_block(j):
                i = j % 2
                def g(dst, src, idx):
                    p.op("pool", lambda e: e.indirect_dma_start(out=dst, out_offset=None, in_=src,
                                                               in_offset=bass.IndirectOffsetOnAxis(ap=idx, axis=0)),
                         reads=[r_idx, r_wgub, r_wdnb], wacc=[r_wt[i]], ch=f"wt{i}")
                g(wgu_t[i][:], wgub, idxw[:, j:j + 1])
                g(wdn_t[i][:], wdnb, idxw[:, j:j + 1])
                g(bgu_t[i][:], bguT, idxw[:, j:j + 1])
                g(bdn_t[i][:], b_dn, idxe[:, j:j + 1])
                dma("sp", xrows[i][:], xs[j * BLK:(j + 1) * BLK, :].rearrange("(a p) d -> p a d", p=128), [r_xs], [r_xrows[i]], ch=f"xrows{i}")

            def transposes(j):
                i = j % 2
                for kc in range(8):
                    pb, r_pb = bank()
                    mm([(pb[:, a * 128:(a + 1) * 128], [(xrows[i][:, a, kc * 128:(kc + 1) * 128], k["ident_b"][:])]) for a in range(4)],
                       [r_xrows[i], kr["ident_b"]], [r_pb])
                    cp("act" if kc % 2 else "dve", xsT[i][:, kc, :], pb[:], [r_pb], [r_xsT[i]])

            load_block(0)
            transposes(0)
            for j in range(NBLK):
                i = j % 2
                if j + 1 < NBLK:
                    load_block(j + 1)
                for fj in range(8):
                    q_ = fj % 2
                    pg_, r_pg = bank()
                    mm([(pg_[:], [(wgu_t[i][:, kc * 2048 + fj * 128:kc * 2048 + (fj + 1) * 128], xsT[i][:, kc, :]) for kc in range(8)])],
                       [r_wt[i], r_xsT[i]], [r_pg])
                    pu_, r_pu = bank()
                    mm([(pu_[:], [(wgu_t[i][:, kc * 2048 + 1024 + fj * 128:kc * 2048 + 1024 + (fj + 1) * 128], xsT[i][:, kc, :]) for kc in range(8)])],
                       [r_wt[i], r_xsT[i]], [r_pu])
                    ts("dve", gc[q_][:], pg_[:], bgu_t[i][:, fj:fj + 1], 7.0, ALU.add, ALU.min, [r_pg, r_wt[i]], [r_gc[q_]])
                    act(sgm[q_][:], gc[q_][:], AF.Sigmoid, [r_gc[q_]], [r_sgm[q_]], scale=1.702)
                    act(uu[q_][:], pu_[:], AF.Identity, [r_pu, r_wt[i]], [r_uu[q_]], bias=bgu_t[i][:, 8 + fj:9 + fj], scale=1.0)
                    ts("dve", uu[q_][:], uu[q_][:], 7.0, -7.0, ALU.min, ALU.max, [r_uu[q_]], [r_uu[q_]])
                    tt("pool", gc[q_][:], gc[q_][:], sgm[q_][:], ALU.mult, [r_gc[q_], r_sgm[q_]], [r_gc[q_]])
                    stt(actT[:, fj, :], uu[q_][:], 1.0, gc[q_][:], ALU.add, ALU.mult, [r_gc[q_], r_uu[q_]], [r_actT[fj]])
                if j + 1 < NBLK:
                    transposes(j + 1)
                for a in range(4):
                    q_ = a % 2
                    for hf in range(2):
                        pb, r_pb = bank()
                        mm([(pb[:], [(actT[:, fc, a * 128:(a + 1) * 128], wdn_t[i][:, fc * 1024 + hf * 512:fc * 1024 + (hf + 1) * 512]) for fc in range(8)])],
                           r_actT + [r_wt[i]], [r_pb])
                        tt("dve", yst[q_][:, hf * 512:(hf + 1) * 512], pb[:], bdn_t[i][:, hf * 512:(hf + 1) * 512], ALU.add, [r_pb, r_wt[i]], [r_yst[q_]])
                    dma("sp", ys[j * BLK + a * 128:j * BLK + (a + 1) * 128, :], yst[q_][:], [r_yst[q_]], [], ch=f"yst{q_}", wacc=[r_ys])

        p.fence()
        with ExitStack() as sc_:
            bc2 = sb(sc_, "bc2", [128, NB + 2, D])
            r_bc2 = R()
            for b in range(NB):
                dma("sp", bc2[:, b, :], modrows[b:b + 1, 40 * 128:48 * 128].partition_broadcast(128), [r_modrows], [r_bc2], ch="bc2")
            dma("sp", bc2[:, NB, :], ln2_g.partition_broadcast(128), [], [r_bc2], ch="bc2")
            dma("sp", bc2[:, NB + 1, :], ln2_b.partition_broadcast(128), [], [r_bc2], ch="bc2")
            yg = [sb(sc_, f"yg{i}", [128, 4, D]) for i in range(2)]
            r_yg = [R(), R()]
            x1r = [sb(sc_, f"x1r{i}", [128, D]) for i in range(2)]
            r_x1r = [R(), R()]
            ffs = [sb(sc_, f"ff{i}", [128, D]) for i in range(2)]
            r_ffs = [R(), R()]
            ot = [sb(sc_, f"ot{i}", [128, D]) for i in range(2)]
            r_ot = [R(), R()]
            stats2s = [sb(sc_, f"stats2{i}", [128, 2, 6]) for i in range(2)]
            mv2s = [sb(sc_, f"mv2{i}", [128, 2]) for i in range(2)]
            nmr2s = [sb(sc_, f"nmr2{i}", [128, 2]) for i in range(2)]
            r_stats2s, r_mv2s, r_nmr2s = [R(), R()], [R(), R()], [R(), R()]
            r_out = R()
            outf = out.rearrange("b l d -> (b l) d")

            def load_c(gt):
                i = gt % 2
                for kk in range(4):
                    p.op("pool", lambda e, kk=kk: e.indirect_dma_start(out=yg[i][:, kk, :], out_offset=None, in_=ys,
                                                                      in_offset=bass.IndirectOffsetOnAxis(ap=desti[:, kk, gt:gt + 1], axis=0)),
                         reads=[r_desti, r_ys], wacc=[r_yg[i]], ch=f"yg{i}")
                dma("sp", x1r[i][:], x1s[gt * 128:(gt + 1) * 128, :], [r_x1s], [r_x1r[i]], ch=f"x1r{i}")

            load_c(0)
            for gt in range(NTT):
                i = gt % 2
                b = gt // NT
                if gt + 1 < NTT:
                    load_c(gt + 1)
                ff, r_ff = ffs[i], r_ffs[i]
                stats2, mv2, nmr2 = stats2s[i], mv2s[i], nmr2s[i]
                r_stats2, r_mv2, r_nmr2 = r_stats2s[i], r_mv2s[i], r_nmr2s[i]
                act(ff[:], yg[i][:, 0, :], AF.Identity, [r_yg[i], r_Wtab], [r_ff], scale=Wtab[:, gt, 0:1])
                for kk in range(1, 4):
                    stt(ff[:], yg[i][:, kk, :], Wtab[:, gt, kk:kk + 1], ff[:], ALU.mult, ALU.add, [r_yg[i], r_Wtab, r_ff], [r_ff])
                tt("dve", ff[:], ff[:], bc2[:, b, :], ALU.mult, [r_ff, r_bc2], [r_ff])
                stt(ff[:], x1r[i][:], ALPHA, ff[:], ALU.mult, ALU.add, [r_x1r[i], r_ff], [r_ff])
                for hf in range(2):
                    p.op("dve", lambda e, hf=hf, stats2=stats2, ff=ff: e.bn_stats(stats2[:, hf, :], ff[:, hf * 512:(hf + 1) * 512]), reads=[r_ff], writes=[r_stats2])
                p.op("dve", lambda e, mv2=mv2, stats2=stats2: e.bn_aggr(mv2[:], stats2[:].rearrange("p a s -> p (a s)")), reads=[r_stats2], writes=[r_mv2])
                act(nmr2[:, 0:1], mv2[:, 1:2], AF.Sqrt, [r_mv2, r_eps], [r_nmr2], bias=epst[:, 1:2], scale=1.0)
                p.op("dve", lambda e, nmr2=nmr2: e.reciprocal(nmr2[:, 0:1], nmr2[:, 0:1]), reads=[r_nmr2], writes=[r_nmr2])
                stt(nmr2[:, 1:2], mv2[:, 0:1], -1.0, nmr2[:, 0:1], ALU.mult, ALU.mult, [r_mv2, r_nmr2], [r_nmr2])
                act(ot[i][:], ff[:], AF.Identity, [r_ff, r_nmr2], [r_ot[i]], bias=nmr2[:, 1:2], scale=nmr2[:, 0:1])
                tt("pool", ot[i][:], ot[i][:], bc2[:, NB, :], ALU.mult, [r_ot[i], r_bc2], [r_ot[i]])
                tt("dve", ot[i][:], ot[i][:], bc2[:, NB + 1, :], ALU.add, [r_ot[i], r_bc2], [r_ot[i]])
                dma("sp", outf[gt * 128:(gt + 1) * 128, :], ot[i][:], [r_ot[i]], [], ch=f"ot{i}", wacc=[r_out])
            p.final_wait("sp", [r_out])
        p.emit()
    return nc


_NC = {}


def kernel(**inputs):
    debug = bool(inputs.pop("_debug", False))
    if debug not in _NC:
        _NC[debug] = build(debug)
    nc = _NC[debug]
    f = lambda a: np.ascontiguousarray(np.asarray(a, dtype=np.float32))
    consts = _consts()
    shared = {
        "w_ada": f(inputs["w_ada"][0]),
        "b_adaT": f(np.asarray(inputs["b_ada"][0]).reshape(48, 128).T),
        "b_ada": f(inputs["b_ada"]),
        "w_in": f(inputs["w_in"][0]),
        "lb_raw": f(inputs["lb_raw"]),
        "normgT": f(np.asarray(inputs["hg_norm_g"][0]).reshape(4, 128).T),
        "w_four_out": f(inputs["w_four_out"][0]),
        "w_hg_out": f(inputs["w_hg_out"][0]),
        "w_o": f(inputs["w_o"][0]),
        "ln1_g": f(inputs["ln1_g"]), "ln1_b": f(inputs["ln1_b"]),
        "w_router": f(inputs["w_router"][0]), "b_router": f(inputs["b_router"]),
        "w_gate_up": f(inputs["w_gate_up"][0]), "b_guT": f(np.asarray(inputs["b_gate_up"][0]).reshape(NE, 16, 128).transpose(0, 2, 1).reshape(NE * 128, 16)),
        "w_down": f(inputs["w_down"][0]), "b_down": f(inputs["b_down"][0]),
        "ln2_g": f(inputs["ln2_g"]), "ln2_b": f(inputs["ln2_b"]),
    }
    for kname, v in consts.items():
        shared["k_" + kname] = v
    x = np.asarray(inputs["x"], dtype=np.float32)
    ctx = np.asarray(inputs["ctx"], dtype=np.float32)
    c = np.asarray(inputs["c"], dtype=np.float32)
    c_ctx = np.asarray(inputs["c_ctx"], dtype=np.float32)
    in_maps = []
    for core in range(NCORES):
        sl = slice(core * NB, (core + 1) * NB)
        c5 = np.concatenate([c[sl], c_ctx[None, :]], axis=0)
        c5 = np.ascontiguousarray(c5.reshape(5, 8, 128).transpose(2, 1, 0))
        m = dict(shared)
        m["x"] = np.ascontiguousarray(x[sl])
        m["ctx"] = np.ascontiguousarray(ctx[sl])
        m["c5"] = c5
        in_maps.append(m)
    res = run_bass_kernel_spmd(nc, in_maps, core_ids=list(range(NCORES)))
    outs = [np.asarray(r["out"], dtype=np.float32) for r in res.results]
    full = np.concatenate(outs, axis=0)
    if debug:
        return full, res.results
    return full
```
